# Optimizing a Trainium2 kernel written in Bass

```python
import jax
import jax.numpy as jnp
from jax import lax
import numpy as np


D_MODEL = 4096
BATCH = 4
SEQ = 4096
DEPTH = 4

MIX_WIDTH = D_MODEL
GROUP_WIDTH = MIX_WIDTH // 4
D_FF = -(-(8 * D_MODEL) // (3 * 256)) * 256
NORM_EPS = 1e-6
ROPE_THETA = 10000.0
CHUNK = 128
Q_BLOCK = 128

MLSTM_HEADS = 4
MLSTM_DQK = GROUP_WIDTH // (2 * MLSTM_HEADS)
MLSTM_DV = GROUP_WIDTH // MLSTM_HEADS

MLA_HEADS = 8
MLA_NOPE = 128
MLA_ROPE = 64
MLA_DV = GROUP_WIDTH // MLA_HEADS
MLA_Q_RANK = 512
MLA_KV_RANK = 512

RET_HEADS = 4
RET_DK = GROUP_WIDTH // (2 * RET_HEADS)
RET_DV = GROUP_WIDTH // RET_HEADS

RWKV_HEAD = 64
RWKV_HEADS = GROUP_WIDTH // RWKV_HEAD
RWKV_DECAY_RANK = 64
RWKV_AAA_RANK = 64
RWKV_GATE_RANK = 160
RWKV_LN_EPS = 64e-5

MLSTM_COLS = (MLSTM_HEADS * MLSTM_DQK, MLSTM_HEADS * MLSTM_DQK, GROUP_WIDTH, GROUP_WIDTH, MLSTM_HEADS, MLSTM_HEADS)
MLA_COLS = (MLA_Q_RANK, MLA_KV_RANK, MLA_ROPE)
RET_COLS = (RET_HEADS * RET_DK, RET_HEADS * RET_DK, GROUP_WIDTH, GROUP_WIDTH)
RWKV_COLS = (GROUP_WIDTH, GROUP_WIDTH, GROUP_WIDTH, RWKV_DECAY_RANK, RWKV_AAA_RANK, RWKV_GATE_RANK)
GROUP_COLS = (sum(MLSTM_COLS), sum(MLA_COLS), sum(RET_COLS), sum(RWKV_COLS))
N_IN_COLS = sum(GROUP_COLS)
RWKV_IN_COLS = sum(RWKV_COLS)

kernel_name = 'hybrid_parallel_heads_trunk'


def split_cols(t, sizes):
    return jnp.split(t, [int(s) for s in np.cumsum(sizes)[:-1]], axis=-1)


def rms_norm(x, gain, eps=NORM_EPS):
    x32 = x.astype(jnp.float32)
    y = x32 * lax.rsqrt(jnp.mean(x32 * x32, axis=-1, keepdims=True) + eps)
    return (y * gain.astype(jnp.float32)).astype(x.dtype)


def layer_norm_heads(x, eps):
    x32 = x.astype(jnp.float32)
    mu = jnp.mean(x32, axis=-1, keepdims=True)
    xc = x32 - mu
    return xc * lax.rsqrt(jnp.mean(xc * xc, axis=-1, keepdims=True) + eps)


def rms_heads(x, eps=NORM_EPS):
    x32 = x.astype(jnp.float32)
    return x32 * lax.rsqrt(jnp.mean(x32 * x32, axis=-1, keepdims=True) + eps)


def apply_rope(x, positions):
    d = x.shape[-1]
    inv_freq = ROPE_THETA ** (-jnp.arange(0, d, 2, dtype=jnp.float32) / d)
    ang = positions.astype(jnp.float32)[..., None] * inv_freq
    cos = jnp.cos(ang)[:, :, None, :]
    sin = jnp.sin(ang)[:, :, None, :]
    x32 = x.astype(jnp.float32)
    x1, x2 = x32[..., : d // 2], x32[..., d // 2:]
    return jnp.concatenate([x1 * cos - x2 * sin, x1 * sin + x2 * cos], axis=-1).astype(x.dtype)


def to_chunks(t):
    B, H, S = t.shape[:3]
    t = t.reshape(B, H, S // CHUNK, CHUNK, *t.shape[3:])
    return jnp.moveaxis(t, 2, 0)


def from_chunks(t):
    t = jnp.moveaxis(t, 0, 2)
    return t.reshape(t.shape[0], t.shape[1], -1, *t.shape[4:])


def mlstm_chunkwise(q, k, v, i_pre, f_pre):
    f32 = jnp.float32
    B, H, S, dqk = q.shape
    dv = v.shape[-1]
    q, k, v = q.astype(f32), k.astype(f32) * dqk ** -0.5, v.astype(f32)
    log_i = i_pre.astype(f32)
    log_f = jax.nn.log_sigmoid(f_pre.astype(f32))
    tril = jnp.tril(jnp.ones((CHUNK, CHUNK), dtype=bool))

    def step(carry, inp):
        C, n, m = carry
        qc, kc, vc, ic, fc = inp
        b = jnp.cumsum(fc, axis=-1)
        D = jnp.where(tril, b[..., :, None] - b[..., None, :] + ic[..., None, :], -jnp.inf)
        inter = b + m[..., None]
        m_t = jnp.maximum(inter, jnp.max(D, axis=-1))
        w_intra = jnp.exp(D - m_t[..., None])
        w_inter = jnp.exp(inter - m_t)
        s = jnp.einsum('bhld,bhmd->bhlm', qc, kc) * w_intra
        num = jnp.einsum('bhlm,bhmv->bhlv', s, vc) + w_inter[..., None] * jnp.einsum('bhld,bhdv->bhlv', qc, C)
        den = jnp.sum(s, axis=-1) + w_inter * jnp.einsum('bhld,bhd->bhl', qc, n)
        h = num / jnp.maximum(jnp.abs(den), jnp.exp(-m_t))[..., None]
        b_end = b[..., -1]
        g = b_end[..., None] - b + ic
        m_new = jnp.maximum(b_end + m, jnp.max(g, axis=-1))
        wk = kc * jnp.exp(g - m_new[..., None])[..., None]
        dec = jnp.exp(b_end + m - m_new)
        C = dec[..., None, None] * C + jnp.einsum('bhmd,bhmv->bhdv', wk, vc)
        n = dec[..., None] * n + jnp.sum(wk, axis=-2)
        return (C, n, m_new), h

    init = (jnp.zeros((B, H, dqk, dv), f32), jnp.zeros((B, H, dqk), f32), jnp.zeros((B, H), f32))
    xs = (to_chunks(q), to_chunks(k), to_chunks(v), to_chunks(log_i), to_chunks(log_f))
    _, h = lax.scan(step, init, xs)
    return from_chunks(h)


def mlstm_mixer(cols, i_bias, f_bias):
    B, S, _ = cols.shape
    q, k, v, o, ig, fg = split_cols(cols, MLSTM_COLS)
    heads = lambda t, d: t.reshape(B, S, MLSTM_HEADS, d).transpose(0, 2, 1, 3)
    ig = (ig + i_bias).transpose(0, 2, 1)
    fg = (fg + f_bias).transpose(0, 2, 1)
    h = mlstm_chunkwise(heads(q, MLSTM_DQK), heads(k, MLSTM_DQK), heads(v, MLSTM_DV), ig, fg)
    h = rms_heads(h).transpose(0, 2, 1, 3).reshape(B, S, GROUP_WIDTH)
    return (jax.nn.sigmoid(o.astype(jnp.float32)) * h).astype(cols.dtype)


def causal_block_attention(q, k, v):
    B, H, S, dqk = q.shape
    dv = v.shape[-1]
    nblk = S // Q_BLOCK
    qb = jnp.moveaxis(q.reshape(B, H, nblk, Q_BLOCK, dqk), 2, 0)
    key_idx = jnp.arange(S)
    scale = dqk ** -0.5

    def one_block(args):
        qi, blk = args
        s = jnp.einsum('bhqd,bhkd->bhqk', qi, k).astype(jnp.float32) * scale
        q_idx = blk * Q_BLOCK + jnp.arange(Q_BLOCK)
        s = jnp.where(key_idx[None, :] <= q_idx[:, None], s, -jnp.inf)
        p = jax.nn.softmax(s, axis=-1).astype(v.dtype)
        return jnp.einsum('bhqk,bhkd->bhqd', p, v)

    out = lax.map(one_block, (qb, jnp.arange(nblk)))
    return jnp.moveaxis(out, 0, 2).reshape(B, H, S, dv)


def mla_mixer(cols, positions, q_norm, kv_norm, w_uq, w_ukv):
    B, S, _ = cols.shape
    cq, ckv, k_pe = split_cols(cols, MLA_COLS)
    q = (rms_norm(cq, q_norm) @ w_uq).reshape(B, S, MLA_HEADS, MLA_NOPE + MLA_ROPE)
    kv = (rms_norm(ckv, kv_norm) @ w_ukv).reshape(B, S, MLA_HEADS, MLA_NOPE + MLA_DV)
    q_nope, q_pe = q[..., :MLA_NOPE], apply_rope(q[..., MLA_NOPE:], positions)
    k_nope, v = kv[..., :MLA_NOPE], kv[..., MLA_NOPE:]
    k_pe = jnp.broadcast_to(apply_rope(k_pe[:, :, None, :], positions), (B, S, MLA_HEADS, MLA_ROPE))
    q = jnp.concatenate([q_nope, q_pe], axis=-1).transpose(0, 2, 1, 3)
    k = jnp.concatenate([k_nope, k_pe], axis=-1).transpose(0, 2, 1, 3)
    o = causal_block_attention(q, k, v.transpose(0, 2, 1, 3))
    return o.transpose(0, 2, 1, 3).reshape(B, S, GROUP_WIDTH).astype(cols.dtype)


def retention_chunkwise(q, k, v):
    f32 = jnp.float32
    B, H, S, dk = q.shape
    dv = v.shape[-1]
    q, k, v = q.astype(f32), k.astype(f32), v.astype(f32)
    log_gamma = jnp.log1p(-jnp.exp2(-5.0 - jnp.arange(H, dtype=f32)))
    idx = jnp.arange(CHUNK, dtype=f32)
    rel = idx[:, None] - idx[None, :]
    decay_mat = jnp.where(rel >= 0, jnp.exp(log_gamma[:, None, None] * jnp.maximum(rel, 0.0)), 0.0)
    q_decay = jnp.exp(log_gamma[:, None] * (idx + 1.0))[..., None]
    k_decay = jnp.exp(log_gamma[:, None] * (CHUNK - 1.0 - idx))[..., None]
    chunk_decay = jnp.exp(log_gamma * CHUNK)[:, None, None]

    def step(R, inp):
        qc, kc, vc = inp
        inner = jnp.einsum('bhld,bhmd->bhlm', qc, kc) * decay_mat
        o = jnp.einsum('bhlm,bhmv->bhlv', inner, vc) + jnp.einsum('bhld,bhdv->bhlv', qc * q_decay, R)
        R = chunk_decay * R + jnp.einsum('bhmd,bhmv->bhdv', kc * k_decay, vc)
        return R, o

    _, o = lax.scan(step, jnp.zeros((B, H, dk, dv), f32), (to_chunks(q), to_chunks(k), to_chunks(v)))
    return from_chunks(o)


def retention_mixer(cols, positions):
    B, S, _ = cols.shape
    q, k, v, g = split_cols(cols, RET_COLS)
    q = apply_rope(q.reshape(B, S, RET_HEADS, RET_DK), positions).transpose(0, 2, 1, 3)
    k = (apply_rope(k.reshape(B, S, RET_HEADS, RET_DK), positions) * RET_DK ** -0.5).transpose(0, 2, 1, 3)
    v = v.reshape(B, S, RET_HEADS, RET_DV).transpose(0, 2, 1, 3)
    o = layer_norm_heads(retention_chunkwise(q, k, v), NORM_EPS)
    o = o.transpose(0, 2, 1, 3).reshape(B, S, GROUP_WIDTH)
    return (jax.nn.silu(g.astype(jnp.float32)) * o).astype(cols.dtype)


def rwkv7_scan(r, w, k, v, a, b):
    f32 = jnp.float32
    B, S, H, d = r.shape

    def step(state, inp):
        rt, wt, kt, vt, at, bt = inp
        sa = jnp.einsum('bhij,bhj->bhi', state, at)
        state = state * wt[:, :, None, :] + sa[..., None] * bt[:, :, None, :] + vt[..., None] * kt[:, :, None, :]
        return state, jnp.einsum('bhij,bhj->bhi', state, rt)

    xs = tuple(jnp.moveaxis(t.astype(f32), 1, 0) for t in (r, w, k, v, a, b))
    _, y = lax.scan(step, jnp.zeros((B, H, d, d), f32), xs)
    return jnp.moveaxis(y, 0, 1)


def rwkv7_mixer(cols, mu, w0, w2, a0, a2, g2, k_k, k_a, r_k, ln_w, ln_b):
    B, S, _ = cols.shape
    prev = jnp.pad(cols, ((0, 0), (1, 0), (0, 0)))[:, :S]
    cols = cols + (prev - cols) * mu
    r, k, v, wd, ad, gd = split_cols(cols, RWKV_COLS)
    w_log = -jax.nn.softplus(-(w0 + jnp.tanh(wd) @ w2)) - 0.5
    decay = jnp.exp(-jnp.exp(w_log.astype(jnp.float32)))
    a = jax.nn.sigmoid(a0 + ad @ a2)
    g = jax.nn.sigmoid(gd) @ g2
    heads = lambda t: t.reshape(B, S, RWKV_HEADS, RWKV_HEAD)
    kk = heads(k * k_k).astype(jnp.float32)
    kk = kk * lax.rsqrt(jnp.maximum(jnp.sum(kk * kk, axis=-1, keepdims=True), 1e-24))
    k = k * (1.0 + (a - 1.0) * k_a)
    rh, kh, vh, ah = heads(r), heads(k), heads(v), heads(a)
    y = rwkv7_scan(rh, heads(decay), kh, vh, -kk, kk * ah)
    y = layer_norm_heads(y, RWKV_LN_EPS).reshape(B, S, GROUP_WIDTH) * ln_w + ln_b
    bonus = jnp.sum(rh * kh * r_k, axis=-1, keepdims=True) * vh
    y = (y + bonus.reshape(B, S, GROUP_WIDTH)) * g
    return y.astype(cols.dtype)


def setup_inputs(seed: int = 0) -> dict:
    key = jax.random.key(seed)
    ks = iter(jax.random.split(key, 32))
    L = DEPTH

    def nrm(shape, scale):
        return jax.random.normal(next(ks), shape, jnp.float32) * scale

    x = nrm((BATCH, SEQ, D_MODEL), 1.0)
    offsets = jax.random.randint(next(ks), (BATCH, 1), 0, 1024, dtype=jnp.int32)
    positions = offsets + jnp.arange(SEQ, dtype=jnp.int32)[None, :]
    return {
        'x': x,
        'positions': positions,
        'attn_norm': 1.0 + nrm((L, D_MODEL), 0.02),
        'w_in': nrm((L, D_MODEL, N_IN_COLS), D_MODEL ** -0.5),
        'mlstm_i_bias': nrm((L, MLSTM_HEADS), 0.1),
        'mlstm_f_bias': jnp.linspace(3.0, 6.0, MLSTM_HEADS, dtype=jnp.float32)[None, :] + nrm((L, MLSTM_HEADS), 0.1),
        'mla_q_norm': 1.0 + nrm((L, MLA_Q_RANK), 0.02),
        'mla_kv_norm': 1.0 + nrm((L, MLA_KV_RANK), 0.02),
        'mla_w_uq': nrm((L, MLA_Q_RANK, MLA_HEADS * (MLA_NOPE + MLA_ROPE)), MLA_Q_RANK ** -0.5),
        'mla_w_ukv': nrm((L, MLA_KV_RANK, MLA_HEADS * (MLA_NOPE + MLA_DV)), MLA_KV_RANK ** -0.5),
        'rwkv_mu': jax.random.uniform(next(ks), (L, RWKV_IN_COLS), jnp.float32),
        'rwkv_w0': nrm((L, GROUP_WIDTH), 0.5),
        'rwkv_w2': nrm((L, RWKV_DECAY_RANK, GROUP_WIDTH), 0.5 * RWKV_DECAY_RANK ** -0.5),
        'rwkv_a0': nrm((L, GROUP_WIDTH), 0.1),
        'rwkv_a2': nrm((L, RWKV_AAA_RANK, GROUP_WIDTH), RWKV_AAA_RANK ** -0.5),
        'rwkv_g2': nrm((L, RWKV_GATE_RANK, GROUP_WIDTH), RWKV_GATE_RANK ** -0.5),
        'rwkv_k_k': 0.85 + nrm((L, GROUP_WIDTH), 0.05),
        'rwkv_k_a': 1.0 + nrm((L, GROUP_WIDTH), 0.05),
        'rwkv_r_k': nrm((L, RWKV_HEADS, RWKV_HEAD), 0.1),
        'rwkv_ln_w': 1.0 + nrm((L, GROUP_WIDTH), 0.02),
        'rwkv_ln_b': nrm((L, GROUP_WIDTH), 0.02),
        'mix_gain': 1.0 + nrm((L, MIX_WIDTH), 0.02),
        'w_out': nrm((L, MIX_WIDTH, D_MODEL), MIX_WIDTH ** -0.5),
        'ffn_norm': 1.0 + nrm((L, D_MODEL), 0.02),
        'w_ffn_gate': nrm((L, D_MODEL, D_FF), D_MODEL ** -0.5),
        'w_ffn_up': nrm((L, D_MODEL, D_FF), D_MODEL ** -0.5),
        'w_ffn_down': nrm((L, D_FF, D_MODEL), D_FF ** -0.5),
        'final_norm': 1.0 + nrm((D_MODEL,), 0.02),
    }


def reference(x, positions, attn_norm, w_in, mlstm_i_bias, mlstm_f_bias, mla_q_norm, mla_kv_norm,
              mla_w_uq, mla_w_ukv, rwkv_mu, rwkv_w0, rwkv_w2, rwkv_a0, rwkv_a2, rwkv_g2, rwkv_k_k,
              rwkv_k_a, rwkv_r_k, rwkv_ln_w, rwkv_ln_b, mix_gain, w_out, ffn_norm, w_ffn_gate,
              w_ffn_up, w_ffn_down, final_norm):
    for l in range(DEPTH):
        h = rms_norm(x, attn_norm[l])
        proj = h @ w_in[l]
        c_mlstm, c_mla, c_ret, c_rwkv = split_cols(proj, GROUP_COLS)
        y_a = mlstm_mixer(c_mlstm, mlstm_i_bias[l], mlstm_f_bias[l])
        y_b = mla_mixer(c_mla, positions, mla_q_norm[l], mla_kv_norm[l], mla_w_uq[l], mla_w_ukv[l])
        y_c = retention_mixer(c_ret, positions)
        y_d = rwkv7_mixer(c_rwkv, rwkv_mu[l], rwkv_w0[l], rwkv_w2[l], rwkv_a0[l], rwkv_a2[l], rwkv_g2[l],
                          rwkv_k_k[l], rwkv_k_a[l], rwkv_r_k[l], rwkv_ln_w[l], rwkv_ln_b[l])
        y = jnp.concatenate([y_a, y_b, y_c, y_d], axis=-1) * mix_gain[l]
        x = x + (y @ w_out[l]).astype(x.dtype)
        h = rms_norm(x, ffn_norm[l])
        x = x + ((jax.nn.silu(h @ w_ffn_gate[l]) * (h @ w_ffn_up[l])) @ w_ffn_down[l]).astype(x.dtype)
    return rms_norm(x, final_norm)
```

```python
import math
import numpy as np
from contextlib import ExitStack
import concourse.bass as bass
import concourse.mybir as mybir
from concourse.bass_utils import run_bass_kernel_spmd

F32 = mybir.dt.float32
BF16 = mybir.dt.bfloat16
I32 = mybir.dt.int32
AF = mybir.ActivationFunctionType
ALU = mybir.AluOpType
AX = mybir.AxisListType

COMPUTE = ('pe', 'act', 'dve', 'pool')
ENGS = ('pe', 'act', 'dve', 'pool', 'sp')
EPOCH = 60000


class Buf:
    __slots__ = ('name', 't', 'w', 'r')

    def __init__(self, name, t=None):
        self.name = name
        self.t = t
        self.w = None
        self.r = {}

    def __getitem__(self, k):
        return self.t[k]


class Prog:
    def __init__(self, nc):
        self.nc = nc
        self.es = ExitStack()
        self.ops = {e: [] for e in ENGS}
        self.count = {e: 0 for e in ENGS}
        self.vc = {e: {} for e in ENGS}
        self.signaled = {e: set() for e in COMPUTE}
        self.dmaval = {}
        self.tokens = {}
        self.nbuf = 0
        self.scopes = [self.es]

    def scope(self):
        prog = self

        class _S:
            def __enter__(s_):
                st = ExitStack()
                prog.scopes.append(st)
                return st

            def __exit__(s_, *a):
                prog.barrier()
                prog.scopes.pop().close()
                return False
        return _S()

    def barrier(self):
        refs = []
        for e in COMPUTE:
            if self.count[e] > 0:
                vc = dict(self.vc[e])
                vc[e] = self.count[e]
                refs.append((e, self.count[e], vc))
        for sk, v in self.dmaval.items():
            refs.append((sk, v, {sk: v}))
        for X in ENGS:
            b = Buf('bar')
            b.r = {i: ref for i, ref in enumerate(refs)}
            waits = self._deps(X, (), [b])
            self.ops[X].append((None, waits, None, None))

    def sb(self, name, shape, dtype):
        self.nbuf += 1
        name = f"{name}_{self.nbuf}"
        t = self.scopes[-1].enter_context(self.nc.sbuf_tensor(name, list(shape), dtype))
        return Buf(name, t)

    def ps(self, name, shape, dtype=F32):
        full = 512 if dtype == F32 else 1024
        self.nbuf += 1
        name = f"{name}_{self.nbuf}"
        t = self.scopes[-1].enter_context(self.nc.psum_tensor(name, [128, full], dtype))
        return Buf(name, t[0:shape[0], 0:shape[1]])

    def tok(self, *key):
        b = self.tokens.get(key)
        if b is None:
            b = Buf(str(key))
            self.tokens[key] = b
        return b

    def _deps(self, eng, r, w, skipkey=None):
        deps = []
        for b in r:
            if b.w is not None:
                deps.append(b.w)
        for b in w:
            if b.w is not None:
                deps.append(b.w)
            deps.extend(b.r.values())
        vc = self.vc[eng]
        waits = []
        for (sk, i, dvc) in deps:
            if sk == eng and eng == 'pe':
                continue
            if skipkey is not None and sk == skipkey:
                continue
            if vc.get(sk, 0) >= i:
                continue
            waits.append((sk, i))
            for k2, v2 in dvc.items():
                if vc.get(k2, 0) < v2:
                    vc[k2] = v2
            vc[sk] = i
            if sk in COMPUTE:
                self.signaled[sk].add(i)
        best = {}
        for sk, i in waits:
            if best.get(sk, 0) < i:
                best[sk] = i
        return list(best.items())

    def op(self, eng, fn, r=(), w=()):
        waits = self._deps(eng, r, w)
        self.count[eng] += 1
        idx = self.count[eng]
        rvc = dict(self.vc[eng])
        rvc[eng] = idx
        ref = (eng, idx, rvc)
        for b in r:
            if b not in w:
                b.r[eng] = ref
        for b in w:
            b.w = ref
            b.r = {}
        self.ops[eng].append((fn, waits, idx, None))
        return ref

    def dma(self, q, out, in_, r=(), w=(), key=None):
        sk = ('d', key)
        waits = self._deps(q, r, w, skipkey=sk)
        v = self.dmaval.get(sk, 0) + 16
        self.dmaval[sk] = v
        rvc = dict(self.vc[q])
        rvc[sk] = v
        ref = (sk, v, rvc)
        for b in r:
            if b not in w:
                b.r[sk] = ref
        for b in w:
            b.w = ref
            b.r = {}
        fn = (lambda e, out=out, in_=in_: e.dma_start(out=out, in_=in_))
        self.ops[q].append((fn, waits, None, sk))
        return ref

    def wait_all(self, eng, bufs):
        waits = self._deps(eng, bufs, ())
        self.ops[eng].append((None, waits, None, None))

    def emit(self):
        nc = self.nc
        es = self.es
        semobj = {}
        valmap = {}
        for e in COMPUTE:
            sig = sorted(self.signaled[e])
            nep = (len(sig) + EPOCH - 1) // EPOCH
            sems = [es.enter_context(nc.semaphore(f"s_{e}_{k}")) for k in range(nep)]
            for pos, i in enumerate(sig):
                valmap[(e, i)] = (sems[pos // EPOCH], pos % EPOCH + 1)
        for n, sk in enumerate(self.dmaval):
            semobj[sk] = es.enter_context(nc.semaphore(f"d_{n}"))

        def run(engname):
            def body(e):
                for fn, waits, idx, dsk in self.ops[engname]:
                    for sk, i in waits:
                        if sk in COMPUTE:
                            s, v = valmap[(sk, i)]
                        else:
                            s, v = semobj[sk], i
                        e.wait_ge(s, v)
                    if fn is None:
                        continue
                    ins = fn(e)
                    if dsk is not None:
                        ins.then_inc(semobj[dsk], 16)
                    elif (engname, idx) in valmap:
                        s, v = valmap[(engname, idx)]
                        ins.then_inc(s, 1)
            return body

        with nc.Block() as block:
            block.tensor(run('pe'))
            block.scalar(run('act'))
            block.vector(run('dve'))
            block.gpsimd(run('pool'))
            block.sync(run('sp'))
        es.close()


D = 4096
DFF = 11008
KC = D // 128
FC = DFF // 128
EPS = 1e-6
FB = 4
WQ = 'pool'


def build_p3(NT, do_mix, final):
    nc = bass.Bass("TRN2", target_bir_lowering=False)
    P = Prog(nc)
    T = 512
    ntile = NT // T
    x_in = nc.dram_tensor("x_in", [NT, D], F32, kind="ExternalInput").ap()
    gnext = nc.dram_tensor("gnext", [128, KC], F32, kind="ExternalInput").ap()
    if final:
        gfin = nc.dram_tensor("gfin", [1, D], F32, kind="ExternalInput").ap()
        out = nc.dram_tensor("out", [NT, D], F32, kind="ExternalOutput").ap()
    else:
        hT_out = nc.dram_tensor("hT_out", [D, NT], BF16, kind="ExternalOutput").ap()
    if do_mix:
        y_in = nc.dram_tensor("y_in", [NT, D], BF16, kind="ExternalInput").ap()
        gmix = nc.dram_tensor("gmix", [128, KC], F32, kind="ExternalInput").ap()
        gffn = nc.dram_tensor("gffn", [128, KC], F32, kind="ExternalInput").ap()
        w_out = nc.dram_tensor("w_out", [D, D], F32, kind="ExternalInput").ap()
        w_g = nc.dram_tensor("w_g", [D, DFF], F32, kind="ExternalInput").ap()
        w_u = nc.dram_tensor("w_u", [D, DFF], F32, kind="ExternalInput").ap()
        w_d = nc.dram_tensor("w_d", [DFF, D], F32, kind="ExternalInput").ap()
        x_out = nc.dram_tensor("x_out", [NT, D], F32, kind="ExternalOutput").ap()

    xs = [P.sb(f"xs{c}", [128, D], F32) for c in range(4)]
    aT = P.sb("aT", [128, KC, T], BF16)
    hs = P.sb("hs", [128, D], BF16)
    ident = P.sb("ident", [128, 128], BF16)
    identf = P.sb("identf", [128, 128], F32)
    g_next = P.sb("g_next", [128, KC], F32)
    ss = P.sb("ss", [128, 8], F32)
    rstd = P.sb("rstd", [128, 8], F32)
    pt = [P.ps(f"pt{i}", [128, 128], BF16) for i in range(2)]
    if final:
        g_fin = P.sb("g_fin", [128, D], F32)
    if do_mix:
        g_mix = P.sb("g_mix", [128, KC], F32)
        g_ffn = P.sb("g_ffn", [128, KC], F32)
        wo = [P.sb(f"wo{i}", [128, KC, 256], BF16) for i in range(2)]
        wgt = [P.sb(f"wg{i}", [128, KC, 128], BF16) for i in range(2)]
        wut = [P.sb(f"wu{i}", [128, KC, 128], BF16) for i in range(2)]
        wdt = [P.sb(f"wd{i}", [128, FB, 512], BF16) for i in range(2)]
        act = [P.sb(f"act{i}", [128, FB, T], BF16) for i in range(2)]
        sg = [P.sb(f"sg{i}", [128, T], F32) for i in range(2)]
        po = [P.ps(f"po{i}", [128, 512]) for i in range(2)]
        pg = [P.ps(f"pg{i}", [128, 512]) for i in range(2)]
        pu = [P.ps(f"pu{i}", [128, 512]) for i in range(2)]

    P.op('pool', lambda e: e.memset(identf[:], 0.0), w=[identf])
    P.op('pool', lambda e: e.affine_select(out=identf[:], in_=identf[:], pattern=[[-1, 128]],
                                           compare_op=ALU.not_equal, fill=1.0, base=0,
                                           channel_multiplier=1), r=[identf], w=[identf])
    P.op('dve', lambda e: e.tensor_copy(out=ident[:], in_=identf[:]), r=[identf], w=[ident])
    P.dma('sp', g_next[:], gnext[:, :], w=[g_next], key='g_next')
    if final:
        P.dma('sp', g_fin[:], gfin.partition_broadcast(128), w=[g_fin], key='g_fin')
    if do_mix:
        P.dma('sp', g_mix[:], gmix[:, :], w=[g_mix], key='g_mix')
        P.dma('sp', g_ffn[:], gffn[:, :], w=[g_ffn], key='g_ffn')

    cnt = {'pt': 0, 'wo': 0, 'gu': 0, 'wd': 0, 'act': 0, 'po': 0, 'pgu': 0, 'sg': 0}

    def transpose_into_aT(src, c, gain):
        for k in range(KC):
            p = pt[cnt['pt'] % 2]
            cnt['pt'] += 1
            P.op('pe', lambda e, p=p, k=k: e.transpose(out=p[:], in_=src[:, k * 128:(k + 1) * 128],
                                                      identity=ident[:]),
                 r=[src, ident], w=[p])
            P.op('dve', lambda e, p=p, k=k: e.tensor_scalar(
                out=aT[:, k, c * 128:(c + 1) * 128], in0=p[:], scalar1=gain[:, k:k + 1],
                scalar2=None, op0=ALU.mult), r=[p, gain], w=[aT])

    def norm_chunk(c):
        P.op('act', lambda e: e.activation(out=hs[:], in_=xs[c][:], func=AF.Square,
                                           accum_out=ss[:, c:c + 1]), r=[xs[c]], w=[hs, ss])
        P.op('act', lambda e: e.activation(out=rstd[:, c:c + 1], in_=ss[:, c:c + 1], func=AF.Sqrt,
                                           scale=1.0 / D, bias=EPS), r=[ss], w=[rstd])
        P.op('dve', lambda e: e.reciprocal(out=rstd[:, c:c + 1], in_=rstd[:, c:c + 1]),
             r=[rstd], w=[rstd])

    for t in range(ntile):
        r0 = t * T
        for c in range(4):
            P.dma('sp', xs[c][:], x_in[r0 + c * 128:r0 + (c + 1) * 128, :], w=[xs[c]],
                  key=f'xs{c}')
        if do_mix:
            for c in range(4):
                P.dma('sp', hs[:], y_in[r0 + c * 128:r0 + (c + 1) * 128, :], w=[hs], key='hs')
                transpose_into_aT(hs, c, g_mix)
            wv = w_out.rearrange("(k p) n -> p k n", p=128)
            for n in range(D // 256):
                w = wo[cnt['wo'] % 2]
                cnt['wo'] += 1
                for q in range(4):
                    P.dma(WQ, w[:, q * 8:(q + 1) * 8, :], wv[:, q * 8:(q + 1) * 8, n * 256:(n + 1) * 256],
                          w=[w], key=w.name)
                for c in range(4):
                    p = po[cnt['po'] % 2]
                    cnt['po'] += 1
                    for k in range(KC):
                        P.op('pe', lambda e, p=p, k=k, c=c, w=w: e.matmul(
                            p[:, 0:256], lhsT=aT[:, k, c * 128:(c + 1) * 128], rhs=w[:, k, :],
                            start=(k == 0), stop=(k == KC - 1)), r=[aT, w], w=[p])
                    P.op('dve', lambda e, p=p, c=c, n=n: e.tensor_tensor(
                        out=xs[c][:, n * 256:(n + 1) * 256], in0=xs[c][:, n * 256:(n + 1) * 256],
                        in1=p[:, 0:256], op=ALU.add), r=[p, xs[c]], w=[xs[c]])
            for c in range(4):
                norm_chunk(c)
                P.op('act', lambda e, c=c: e.activation(out=hs[:], in_=xs[c][:], func=AF.Identity,
                                                        scale=rstd[:, c:c + 1]),
                     r=[xs[c], rstd], w=[hs])
                transpose_into_aT(hs, c, g_ffn)
            wgv = w_g.rearrange("(k p) n -> p k n", p=128)
            wuv = w_u.rearrange("(k p) n -> p k n", p=128)
            wdv = w_d.rearrange("(j p) n -> p j n", p=128)
            for j0 in range(0, FC, FB):
                nb = min(FB, FC - j0)
                a = act[cnt['act'] % 2]
                cnt['act'] += 1
                for jj in range(nb):
                    j = j0 + jj
                    s = cnt['gu'] % 2
                    cnt['gu'] += 1
                    for q in range(2):
                        P.dma(WQ, wgt[s][:, q * 16:(q + 1) * 16, :],
                              wgv[:, q * 16:(q + 1) * 16, j * 128:(j + 1) * 128], w=[wgt[s]], key=wgt[s].name)
                        P.dma(WQ, wut[s][:, q * 16:(q + 1) * 16, :],
                              wuv[:, q * 16:(q + 1) * 16, j * 128:(j + 1) * 128], w=[wut[s]], key=wut[s].name)
                    s2 = cnt['pgu'] % 2
                    cnt['pgu'] += 1
                    for k in range(KC):
                        P.op('pe', lambda e, s=s, s2=s2, k=k: e.matmul(
                            pg[s2][:], lhsT=wgt[s][:, k, :], rhs=aT[:, k, :],
                            start=(k == 0), stop=(k == KC - 1)), r=[wgt[s], aT], w=[pg[s2]])
                    for k in range(KC):
                        P.op('pe', lambda e, s=s, s2=s2, k=k: e.matmul(
                            pu[s2][:], lhsT=wut[s][:, k, :], rhs=aT[:, k, :],
                            start=(k == 0), stop=(k == KC - 1)), r=[wut[s], aT], w=[pu[s2]])
                    s3 = cnt['sg'] % 2
                    cnt['sg'] += 1
                    P.op('act', lambda e, s2=s2, s3=s3: e.activation(out=sg[s3][:], in_=pg[s2][:],
                                                                   func=AF.Silu),
                         r=[pg[s2]], w=[sg[s3]])
                    P.op('dve', lambda e, s2=s2, s3=s3, a=a, jj=jj: e.tensor_tensor(
                        out=a[:, jj, :], in0=sg[s3][:], in1=pu[s2][:], op=ALU.mult),
                         r=[sg[s3], pu[s2]], w=[a])
                for n in range(D // 512):
                    s = cnt['wd'] % 2
                    cnt['wd'] += 1
                    P.dma(WQ, wdt[s][:, 0:nb, :], wdv[:, j0:j0 + nb, n * 512:(n + 1) * 512],
                          w=[wdt[s]], key=wdt[s].name)
                    for c in range(4):
                        p = po[cnt['po'] % 2]
                        cnt['po'] += 1
                        for jj in range(nb):
                            P.op('pe', lambda e, p=p, jj=jj, c=c, s=s, a=a, nb=nb: e.matmul(
                                p[:], lhsT=a[:, jj, c * 128:(c + 1) * 128], rhs=wdt[s][:, jj, :],
                                start=(jj == 0), stop=(jj == nb - 1)), r=[a, wdt[s]], w=[p])
                        P.op('dve', lambda e, p=p, c=c, n=n: e.tensor_tensor(
                            out=xs[c][:, n * 512:(n + 1) * 512], in0=xs[c][:, n * 512:(n + 1) * 512],
                            in1=p[:], op=ALU.add), r=[p, xs[c]], w=[xs[c]])
            for c in range(4):
                P.dma('sp', x_out[r0 + c * 128:r0 + (c + 1) * 128, :], xs[c][:], r=[xs[c]],
                      w=[P.tok('x_out', t, c)], key=f'xs{c}')
        for c in range(4):
            norm_chunk(c)
            if final:
                P.op('dve', lambda e, c=c: e.scalar_tensor_tensor(
                    out=xs[c][:], in0=xs[c][:], scalar=rstd[:, c:c + 1], in1=g_fin[:],
                    op0=ALU.mult, op1=ALU.mult), r=[xs[c], rstd, g_fin], w=[xs[c]])
                P.dma('sp', out[r0 + c * 128:r0 + (c + 1) * 128, :], xs[c][:], r=[xs[c]],
                      w=[P.tok('out', t, c)], key=f'xs{c}')
            else:
                P.op('act', lambda e, c=c: e.activation(out=hs[:], in_=xs[c][:], func=AF.Identity,
                                                        scale=rstd[:, c:c + 1]),
                     r=[xs[c], rstd], w=[hs])
                transpose_into_aT(hs, c, g_next)
        if not final:
            hv = hT_out.rearrange("(k p) n -> p k n", p=128)
            P.dma('sp', hv[:, :, r0:r0 + T], aT[:], r=[aT], w=[P.tok('hT', t)], key='aT')
    P.wait_all('sp', list(P.tokens.values()))
    P.emit()
    return nc


C_L = 1540
C_W = 4164
NW = 1824
LDEC = -math.exp(-0.5)


def mla_phase(P, L):
    S, NCH, proj, y_out = L['S'], L['NCH'], L['proj'], L['y_out']
    proj_toks, mk_consts, rope_tables, rope_tm = L['proj_toks'], L['mk_consts'], L['rope_tables'], L['rope_tm']
    qn_c, kvn_c, w_uq, w_ukv = L['qn_c'], L['kvn_c'], L['w_uq'], L['w_ukv']
    SC = 192.0 ** -0.5
    with P.scope():
        cst = mk_consts()
        ident, identf, triu = cst['ident'], cst['identf'], cst['triu']
        cs, sn = rope_tables(32, 64, "l")
        pl = [P.sb(f"pl{i}", [128, 1088], F32) for i in range(2)]
        wq = P.sb("wq", [128, 4, 768], BF16)
        wkv = P.sb("wkv", [128, 4, 1024], BF16)
        gq = P.sb("gq", [128, 4], F32)
        gkv = P.sb("gkv", [128, 4], F32)
        KT = P.sb("KT", [128, 4, S], BF16)
        KPT = P.sb("KPT", [128, S], BF16)
        Vc = P.sb("Vc", [128, NCH, 512], BF16)
        cn = P.sb("cn", [128, 1024], BF16)
        cT = P.sb("cT", [128, 8, 128], BF16)
        qnT = P.sb("qnT", [128, 4, 128], BF16)
        qpe = P.sb("qpe", [128, 256], F32)
        qper = P.sb("qper", [128, 256], F32)
        qpT = P.sb("qpT", [128, 2, 128], BF16)
        kpe = P.sb("kpe", [128, 128], F32)
        tmp = P.sb("tmp", [128, 512], F32)
        sc = P.sb("sc", [128, S], F32)
        Pb = P.sb("Pb", [128, S], BF16)
        PTb = [P.sb(f"PTb{i}", [128, 128], BF16) for i in range(3)]
        sm = P.sb("sm", [128, 8], F32)
        junk = P.sb("junk", [128, 512], BF16)
        yb = [P.sb(f"yb{i}", [128, 512], BF16) for i in range(2)]
        cmask = P.sb("cmask", [128, 128], F32)
        pA = [P.ps(f"pA{i}", [128, 512]) for i in range(2)]
        pT = [P.ps(f"pT{i}", [128, 128], BF16) for i in range(2)]
        pTf = P.ps("pTf", [128, 128])
        pS = [P.ps(f"pS{i}", [128, 512]) for i in range(2)]
        pO = P.ps("pO", [128, 128])
        P.dma('pool', wq[:], w_uq.rearrange("(k p) n -> p k n", p=128), w=[wq], key='wq')
        P.dma('pool', wkv[:], w_ukv.rearrange("(k p) n -> p k n", p=128), w=[wkv], key='wkv')
        P.dma('sp', gq[:], qn_c[:, :], w=[gq], key='gq')
        P.dma('sp', gkv[:], kvn_c[:, :], w=[gkv], key='gkv')
        P.op('pool', lambda e: e.memset(cmask[:], 0.0), w=[cmask])
        P.op('pool', lambda e: e.affine_select(out=cmask[:], in_=cmask[:], pattern=[[-1, 128]],
                                               compare_op=ALU.is_ge, fill=-30000.0, base=0,
                                               channel_multiplier=1), r=[cmask], w=[cmask])
        npt = 0
        for c in range(NCH):
            p_ = pl[c % 2]
            P.dma('sp', p_[:], proj[c * 128:(c + 1) * 128, C_L:C_L + 1088], r=proj_toks(c), w=[p_], key=p_.name)
            for g in range(2):
                P.op('act', lambda e, g=g, p_=p_: e.activation(out=junk[:], in_=p_[:, g * 512:(g + 1) * 512],
                                                              func=AF.Square, accum_out=sm[:, g:g + 1]),
                     r=[p_], w=[junk, sm])
                P.op('act', lambda e, g=g: e.activation(out=sm[:, g:g + 1], in_=sm[:, g:g + 1], func=AF.Sqrt,
                                                        scale=1.0 / 512, bias=1e-6), r=[sm], w=[sm])
                P.op('dve', lambda e, g=g: e.reciprocal(out=sm[:, g:g + 1], in_=sm[:, g:g + 1]), r=[sm], w=[sm])
                P.op('act', lambda e, g=g, p_=p_: e.activation(out=cn[:, g * 512:(g + 1) * 512],
                                                              in_=p_[:, g * 512:(g + 1) * 512], func=AF.Identity,
                                                              scale=sm[:, g:g + 1]), r=[p_, sm], w=[cn])
                gain = gq if g == 0 else gkv
                for k in range(4):
                    p = pT[npt % 2]
                    npt += 1
                    P.op('pe', lambda e, p=p, g=g, k=k: e.transpose(
                        out=p[:], in_=cn[:, g * 512 + k * 128:g * 512 + (k + 1) * 128], identity=ident[:]),
                         r=[cn, ident], w=[p])
                    P.op('dve', lambda e, p=p, g=g, k=k, gain=gain: e.tensor_scalar(
                        out=cT[:, g * 4 + k, :], in0=p[:], scalar1=gain[:, k:k + 1], scalar2=None, op0=ALU.mult),
                         r=[p, gain], w=[cT])
            for h in range(4):
                pa = pA[h % 2]
                for k in range(4):
                    P.op('pe', lambda e, pa=pa, h=h, k=k: e.matmul(
                        pa[:, 0:128], lhsT=wq[:, k, h * 192:h * 192 + 128], rhs=cT[:, k, :], start=(k == 0),
                        stop=(k == 3)), r=[wq, cT], w=[pa])
                P.op('act', lambda e, pa=pa, h=h: e.activation(out=qnT[:, h, :], in_=pa[:, 0:128], func=AF.Identity,
                                                              scale=SC), r=[pa], w=[qnT])
                pa2 = pS[h % 2]
                for k in range(4):
                    P.op('pe', lambda e, pa2=pa2, h=h, k=k: e.matmul(
                        pa2[:, 0:128], lhsT=wkv[:, k, h * 256:h * 256 + 128], rhs=cT[:, 4 + k, :], start=(k == 0),
                        stop=(k == 3)), r=[wkv, cT], w=[pa2])
                P.op('dve', lambda e, pa2=pa2, h=h, c=c: e.tensor_copy(out=KT[:, h, c * 128:(c + 1) * 128],
                                                                      in_=pa2[:, 0:128]), r=[pa2], w=[KT])
            pa = pA[0]
            wv_ = wkv[:, :, :].rearrange("p k (h c) -> p k h c", c=256)
            for k in range(4):
                P.op('pe', lambda e, pa=pa, k=k: e.matmul(
                    pa[:, :].rearrange("p (h c) -> p h c", c=128), lhsT=cT[:, 4 + k, :], rhs=wv_[:, k, :, 128:256],
                    start=(k == 0), stop=(k == 3)), r=[wkv, cT], w=[pa])
            P.op('act', lambda e, pa=pa, c=c: e.activation(out=Vc[:, c, :], in_=pa[:, :], func=AF.Identity),
                 r=[pa], w=[Vc])
            pa = pA[1]
            wq_ = wq[:, :, :].rearrange("p k (h c) -> p k h c", c=192)
            for k in range(4):
                P.op('pe', lambda e, pa=pa, k=k: e.matmul(
                    pa[:, 0:256].rearrange("p (h c) -> p h c", c=64), lhsT=cT[:, k, :], rhs=wq_[:, k, :, 128:192],
                    start=(k == 0), stop=(k == 3)), r=[wq, cT], w=[pa])
            P.op('act', lambda e, pa=pa: e.activation(out=qpe[:], in_=pa[:, 0:256], func=AF.Identity, scale=SC),
                 r=[pa], w=[qpe])
            for f in rope_tm(qpe[:], qper[:], 4, 32, cs, sn, c, tmp):
                P.op('dve', f, r=[qpe, cs, sn, tmp, qper], w=[tmp, qper])
            for hp in range(2):
                P.op('pe', lambda e, hp=hp: e.transpose(out=pTf[:], in_=qper[:, hp * 128:(hp + 1) * 128],
                                                        identity=identf[:]), r=[qper, identf], w=[pTf])
                P.op('act', lambda e, hp=hp: e.activation(out=qpT[:, hp, :], in_=pTf[:], func=AF.Identity),
                     r=[pTf], w=[qpT])
            for f in rope_tm(p_[:, 1024:1088], kpe[:, 0:64], 1, 32, cs, sn, c, tmp):
                P.op('dve', f, r=[p_, cs, sn, tmp, kpe], w=[tmp, kpe])
            P.op('dve', lambda e: e.tensor_copy(out=kpe[:, 64:128], in_=kpe[:, 0:64]), r=[kpe], w=[kpe])
            P.op('pe', lambda e: e.transpose(out=pTf[:], in_=kpe[:], identity=identf[:]), r=[kpe, identf], w=[pTf])
            P.op('act', lambda e, c=c: e.activation(out=KPT[:, c * 128:(c + 1) * 128], in_=pTf[:], func=AF.Identity),
                 r=[pTf], w=[KPT])
            nk = (c + 1) * 128
            for h in range(4):
                pb = (h % 2) * 64
                for k0 in range(0, nk, 512):
                    kn = min(512, nk - k0)
                    ps = pS[(k0 // 512) % 2]
                    P.op('pe', lambda e, ps=ps, h=h, k0=k0, kn=kn: e.matmul(
                        ps[:, 0:kn], lhsT=qnT[:, h, :], rhs=KT[:, h, k0:k0 + kn], start=True, stop=False),
                         r=[qnT, KT], w=[ps])
                    P.op('pe', lambda e, ps=ps, h=h, k0=k0, kn=kn, pb=pb: e.matmul(
                        ps[:, 0:kn], lhsT=qpT[pb:pb + 64, h // 2, :], rhs=KPT[pb:pb + 64, k0:k0 + kn],
                        start=False, stop=True), r=[qpT, KPT], w=[ps])
                    P.op('act', lambda e, ps=ps, k0=k0, kn=kn: e.activation(out=sc[:, k0:k0 + kn], in_=ps[:, 0:kn],
                                                                           func=AF.Identity), r=[ps], w=[sc])
                P.op('dve', lambda e, nk=nk: e.tensor_tensor(out=sc[:, nk - 128:nk], in0=sc[:, nk - 128:nk],
                                                            in1=cmask[:], op=ALU.add), r=[sc, cmask], w=[sc])
                P.op('dve', lambda e, nk=nk: e.tensor_reduce(out=sm[:, 4:5], in_=sc[:, 0:nk], axis=AX.X, op=ALU.max),
                     r=[sc], w=[sm])
                P.op('dve', lambda e: e.tensor_scalar(out=sm[:, 4:5], in0=sm[:, 4:5], scalar1=-1.0, scalar2=None,
                                                      op0=ALU.mult), r=[sm], w=[sm])
                P.op('act', lambda e, nk=nk: e.activation(out=Pb[:, 0:nk], in_=sc[:, 0:nk], func=AF.Exp,
                                                         bias=sm[:, 4:5], accum_out=sm[:, 5:6]),
                     r=[sc, sm], w=[Pb, sm])
                P.op('dve', lambda e: e.reciprocal(out=sm[:, 5:6], in_=sm[:, 5:6]), r=[sm], w=[sm])
                for kb in range(c + 1):
                    p = pT[npt % 2]
                    pt_ = PTb[npt % 3]
                    npt += 1
                    P.op('pe', lambda e, p=p, kb=kb: e.transpose(out=p[:], in_=Pb[:, kb * 128:(kb + 1) * 128],
                                                                identity=ident[:]), r=[Pb, ident], w=[p])
                    if kb % 2 == 0:
                        P.op('dve', lambda e, p=p, pt_=pt_: e.tensor_copy(out=pt_[:], in_=p[:]), r=[p], w=[pt_])
                    else:
                        P.op('act', lambda e, p=p, pt_=pt_: e.activation(out=pt_[:], in_=p[:], func=AF.Identity),
                             r=[p], w=[pt_])
                    P.op('pe', lambda e, pt_=pt_, kb=kb, h=h, c=c: e.matmul(
                        pO[:], lhsT=pt_[:], rhs=Vc[:, kb, h * 128:(h + 1) * 128], start=(kb == 0), stop=(kb == c)),
                         r=[pt_, Vc], w=[pO])
                P.op('dve', lambda e, h=h, c=c: e.tensor_scalar(out=yb[c % 2][:, h * 128:(h + 1) * 128], in0=pO[:],
                                                               scalar1=sm[:, 5:6], scalar2=None, op0=ALU.mult),
                     r=[pO, sm], w=[yb[c % 2]])
            P.dma('sp', y_out[c * 128:(c + 1) * 128, 512:1024], yb[c % 2][:], r=[yb[c % 2]],
                  w=[P.tok('y', 'mla', c)], key=yb[c % 2].name)


def rwkv_phase(P, L):
    S, NCH, proj, y_out = L['S'], L['NCH'], L['proj'], L['y_out']
    proj_toks, mk_consts = L['proj_toks'], L['mk_consts']
    rw_mu, rw_vec, rw_w2, rw_a2, rw_g2 = L['rw_mu'], L['rw_vec'], L['rw_w2'], L['rw_a2'], L['rw_g2']
    with P.scope():
        cst = mk_consts()
        ident, identf, triu, trius, tril_s, ones = (cst[k] for k in ('ident', 'identf', 'triu', 'trius', 'tril_s', 'ones'))
        xc = [P.sb(f"xc{i}", [128, NW], F32) for i in range(2)]
        xp = [P.sb(f"xp{i}", [128, NW], F32) for i in range(2)]
        xs = P.sb("xs", [128, NW], F32)
        mu = P.sb("mu", [128, NW], F32)
        vec = P.sb("vec", [128, 7, 512], F32)
        oka = P.sb("oka", [128, 512], F32)
        w2 = P.sb("w2", [128, 512], F32)
        g2 = P.sb("g2", [128, 2, 512], F32)
        lr = P.sb("lr", [128, 384], F32)
        lrT = P.sb("lrT", [128, 3, 128], F32)
        names = ['sig', 'aa', 'gg', 'kk', 'k2', 'bv', 'Winc', 'Winv', 'Wexc', 'Wend', 't0', 't1', 'csp']
        T_ = {n: P.sb("rw_" + n, [128, 512], F32) for n in names}
        mask2 = P.sb("mask2", [128, 256], F32)
        ART = P.sb("ART", [128, 4, 2, 128], BF16)
        bT = P.sb("bT", [128, 4, 128], BF16)
        kTt = P.sb("kTt", [128, 4, 128], BF16)
        Bh = P.sb("Bh", [128, 512], BF16)
        Kh = P.sb("Kh", [128, 512], BF16)
        Vb = P.sb("Vb", [128, 512], BF16)
        UU = P.sb("UU", [128, 512], BF16)
        Mb = P.sb("Mb", [128, 256], BF16)
        Mk = P.sb("Mk", [128, 256], BF16)
        NT = P.sb("NT", [128, 128], BF16)
        Pm = [P.sb(f"Pm{i}", [128, 128], BF16) for i in range(2)]
        PmT = [P.sb(f"PmT{i}", [128, 128], BF16) for i in range(2)]
        X = [P.sb(f"X{i}", [128, 128], BF16) for i in range(2)]
        Zs = P.sb("Zs", [128, 64], BF16)
        Tst = [P.sb(f"Tst{i}", [128, 64], F32) for i in range(4)]
        Tbf = [P.sb(f"Tbf{i}", [128, 64], BF16) for i in range(4)]
        WLc = P.sb("WLc", [128, 4], F32)
        Ys = P.sb("Ys", [128, 512], F32)
        sm = P.sb("sm", [128, 32], F32)
        yb = [P.sb(f"yb{i}", [128, 512], BF16) for i in range(2)]
        pA = [P.ps(f"pA{i}", [128, 512]) for i in range(2)]
        pT = [P.ps(f"pT{i}", [128, 128]) for i in range(2)]
        pM = [P.ps(f"pM{i}", [128, 256]) for i in range(2)]
        pQ = [P.ps(f"pQ{i}", [128, 128]) for i in range(2)]
        P.dma('sp', mu[:], rw_mu.partition_broadcast(128), w=[mu], key='mu')
        for i in range(7):
            P.dma('sp', vec[:, i, :], rw_vec[i:i + 1, :].partition_broadcast(128), w=[vec], key='vec')
        P.op('dve', lambda e: e.tensor_scalar(out=oka[:], in0=vec[:, 3, :], scalar1=-1.0, scalar2=1.0,
                                              op0=ALU.mult, op1=ALU.add), r=[vec], w=[oka])
        P.dma('sp', w2[0:64, :], rw_w2[:, :], w=[w2], key='w2')
        P.dma('sp', w2[64:128, :], rw_a2[:, :], w=[w2], key='w2')
        P.dma('sp', g2[:, 0, :], rw_g2[0:128, :], w=[g2], key='g2')
        P.dma('sp', g2[0:32, 1, :], rw_g2[128:160, :], w=[g2], key='g2')
        P.op('dve', lambda e: e.tensor_copy(out=mask2[:, 0:128], in_=trius[:]), r=[trius], w=[mask2])
        P.op('dve', lambda e: e.tensor_copy(out=mask2[:, 128:256], in_=triu[:]), r=[triu], w=[mask2])
        for i in range(4):
            P.op('dve', lambda e, i=i: e.memset(Tst[i][:], 0.0), w=[Tst[i]])
            P.op('dve', lambda e, i=i: e.memset(Tbf[i][:], 0.0), w=[Tbf[i]])
        P.op('dve', lambda e: e.memset(lr[:], 0.0), w=[lr])
        w0b, a0b, kkb, kab, lnw, lnb, rkb = (vec[:, i, :] for i in range(7))
        V3 = lambda ap: ap.rearrange("p (h c) -> p h c", c=64)
        B3 = lambda ap: ap.unsqueeze(2).to_broadcast([128, 8, 64])
        ntp = 0
        for c in range(NCH):
            x_, xp_ = xc[c % 2], xp[c % 2]
            P.dma('sp', x_[:], proj[c * 128:(c + 1) * 128, C_W:C_W + NW], r=proj_toks(c), w=[x_], key=x_.name)
            if c == 0:
                P.op('dve', lambda e, xp_=xp_: e.memset(xp_[0:1, :], 0.0), w=[xp_])
                P.dma('sp', xp_[1:128, :], proj[0:127, C_W:C_W + NW], r=proj_toks(0), w=[xp_], key=xp_.name)
            else:
                P.dma('sp', xp_[:], proj[c * 128 - 1:(c + 1) * 128 - 1, C_W:C_W + NW],
                      r=proj_toks(c) + proj_toks(c - 1), w=[xp_], key=xp_.name)
            P.op('dve', lambda e, x_=x_, xp_=xp_: e.tensor_tensor(out=xs[:], in0=xp_[:], in1=x_[:], op=ALU.subtract),
                 r=[x_, xp_], w=[xs])
            P.op('dve', lambda e: e.tensor_tensor(out=xs[:], in0=xs[:], in1=mu[:], op=ALU.mult), r=[xs, mu], w=[xs])
            P.op('dve', lambda e, x_=x_: e.tensor_tensor(out=xs[:], in0=xs[:], in1=x_[:], op=ALU.add), r=[xs, x_], w=[xs])
            r_, k_, v_ = xs[:, 0:512], xs[:, 512:1024], xs[:, 1024:1536]
            P.op('act', lambda e: e.activation(out=lr[:, 0:64], in_=xs[:, 1536:1600], func=AF.Tanh), r=[xs], w=[lr])
            P.op('act', lambda e: e.activation(out=lr[:, 64:128], in_=xs[:, 1600:1664], func=AF.Identity), r=[xs], w=[lr])
            P.op('act', lambda e: e.activation(out=lr[:, 128:288], in_=xs[:, 1664:1824], func=AF.Sigmoid), r=[xs], w=[lr])
            for i in range(3):
                p = pT[ntp % 2]
                ntp += 1
                P.op('pe', lambda e, p=p, i=i: e.transpose(out=p[:], in_=lr[:, i * 128:(i + 1) * 128], identity=identf[:]),
                     r=[lr, identf], w=[p])
                P.op('act', lambda e, p=p, i=i: e.activation(out=lrT[:, i, :], in_=p[:], func=AF.Identity), r=[p], w=[lrT])
            sig, aa, gg, kk, k2, bv = (T_[n] for n in ('sig', 'aa', 'gg', 'kk', 'k2', 'bv'))
            Winc, Winv, Wexc, Wend, t0, t1, csp = (T_[n] for n in ('Winc', 'Winv', 'Wexc', 'Wend', 't0', 't1', 'csp'))
            P.op('pe', lambda e: e.matmul(pA[0][:], lhsT=lrT[0:64, 0, :], rhs=w2[0:64, :], start=True, stop=True),
                 r=[lrT, w2], w=[pA[0]])
            P.op('dve', lambda e: e.tensor_tensor(out=sig[:], in0=pA[0][:], in1=w0b, op=ALU.add), r=[pA[0], vec], w=[sig])
            P.op('act', lambda e: e.activation(out=sig[:], in_=sig[:], func=AF.Sigmoid), r=[sig], w=[sig])
            P.op('pe', lambda e: e.matmul(pA[1][:], lhsT=lrT[64:128, 0, :], rhs=w2[64:128, :], start=True, stop=True),
                 r=[lrT, w2], w=[pA[1]])
            P.op('dve', lambda e: e.tensor_tensor(out=aa[:], in0=pA[1][:], in1=a0b, op=ALU.add), r=[pA[1], vec], w=[aa])
            P.op('act', lambda e: e.activation(out=aa[:], in_=aa[:], func=AF.Sigmoid), r=[aa], w=[aa])
            P.op('pe', lambda e: e.matmul(pA[0][:], lhsT=lrT[:, 1, :], rhs=g2[:, 0, :], start=True, stop=False),
                 r=[lrT, g2], w=[pA[0]])
            P.op('pe', lambda e: e.matmul(pA[0][:], lhsT=lrT[0:32, 2, :], rhs=g2[0:32, 1, :], start=False, stop=True),
                 r=[lrT, g2], w=[pA[0]])
            P.op('act', lambda e: e.activation(out=gg[:], in_=pA[0][:], func=AF.Identity), r=[pA[0]], w=[gg])
            P.op('dve', lambda e: e.tensor_tensor(out=kk[:], in0=k_, in1=kkb, op=ALU.mult), r=[xs, vec], w=[kk])
            P.op('dve', lambda e: e.tensor_tensor(out=t0[:], in0=kk[:], in1=kk[:], op=ALU.mult), r=[kk], w=[t0])
            P.op('dve', lambda e: e.tensor_reduce(out=sm[:, 0:8], in_=V3(t0[:]), axis=AX.X, op=ALU.add), r=[t0], w=[sm])
            P.op('act', lambda e: e.activation(out=sm[:, 0:8], in_=sm[:, 0:8], func=AF.Sqrt), r=[sm], w=[sm])
            P.op('dve', lambda e: e.tensor_scalar(out=sm[:, 0:8], in0=sm[:, 0:8], scalar1=1e-12, scalar2=None,
                                                  op0=ALU.max), r=[sm], w=[sm])
            P.op('dve', lambda e: e.reciprocal(out=sm[:, 0:8], in_=sm[:, 0:8]), r=[sm], w=[sm])
            P.op('dve', lambda e: e.tensor_tensor(out=V3(kk[:]), in0=V3(kk[:]), in1=B3(sm[:, 0:8]), op=ALU.mult),
                 r=[kk, sm], w=[kk])
            P.op('dve', lambda e: e.tensor_tensor(out=k2[:], in0=aa[:], in1=kab, op=ALU.mult), r=[aa, vec], w=[k2])
            P.op('dve', lambda e: e.tensor_tensor(out=k2[:], in0=k2[:], in1=oka[:], op=ALU.add), r=[k2, oka], w=[k2])
            P.op('dve', lambda e: e.tensor_tensor(out=k2[:], in0=k2[:], in1=k_, op=ALU.mult), r=[k2, xs], w=[k2])
            P.op('dve', lambda e: e.tensor_tensor(out=bv[:], in0=kk[:], in1=aa[:], op=ALU.mult), r=[kk, aa], w=[bv])
            P.op('pe', lambda e: e.matmul(pA[1][:], lhsT=triu[:], rhs=sig[:], start=True, stop=True), r=[triu, sig], w=[pA[1]])
            P.op('act', lambda e: e.activation(out=csp[:], in_=pA[1][:], func=AF.Identity), r=[pA[1]], w=[csp])
            P.op('act', lambda e: e.activation(out=Winc[:], in_=csp[:], func=AF.Exp, scale=LDEC), r=[csp], w=[Winc])
            P.op('act', lambda e: e.activation(out=Winv[:], in_=csp[:], func=AF.Exp, scale=-LDEC), r=[csp], w=[Winv])
            P.op('dve', lambda e: e.tensor_tensor(out=t0[:], in0=csp[:], in1=sig[:], op=ALU.subtract), r=[csp, sig], w=[t0])
            P.op('act', lambda e: e.activation(out=Wexc[:], in_=t0[:], func=AF.Exp, scale=LDEC), r=[t0], w=[Wexc])
            P.op('pe', lambda e: e.matmul(pA[0][:], lhsT=ones[:], rhs=sig[:], start=True, stop=True), r=[ones, sig], w=[pA[0]])
            P.op('act', lambda e: e.activation(out=t1[:], in_=pA[0][:], func=AF.Identity), r=[pA[0]], w=[t1])
            P.op('dve', lambda e: e.tensor_tensor(out=t0[:], in0=t1[:], in1=csp[:], op=ALU.subtract), r=[t1, csp], w=[t0])
            P.op('act', lambda e: e.activation(out=Wend[:], in_=t0[:], func=AF.Exp, scale=LDEC), r=[t0], w=[Wend])
            for hp in range(4):
                P.op('pe', lambda e, hp=hp: e.matmul(pQ[0][:, hp:hp + 1], lhsT=sig[:, hp * 128:(hp + 1) * 128],
                                                     rhs=ones[:, 0:1], start=True, stop=True), r=[sig, ones], w=[pQ[0]])
            P.op('act', lambda e: e.activation(out=WLc[:], in_=pQ[0][:, 0:4], func=AF.Exp, scale=LDEC), r=[pQ[0]], w=[WLc])
            P.op('dve', lambda e: e.tensor_tensor(out=Bh[:], in0=bv[:], in1=Wend[:], op=ALU.mult), r=[bv, Wend], w=[Bh])
            P.op('dve', lambda e: e.tensor_tensor(out=Kh[:], in0=k2[:], in1=Wend[:], op=ALU.mult), r=[k2, Wend], w=[Kh])
            P.op('act', lambda e: e.activation(out=Vb[:], in_=v_, func=AF.Identity), r=[xs], w=[Vb])
            P.op('dve', lambda e: e.tensor_tensor(out=Winc[:], in0=Winc[:], in1=r_, op=ALU.mult), r=[Winc, xs], w=[Winc])
            P.op('dve', lambda e: e.scalar_tensor_tensor(out=Wexc[:], in0=Wexc[:], scalar=-1.0, in1=kk[:], op0=ALU.mult,
                                                         op1=ALU.mult), r=[Wexc, kk], w=[Wexc])
            P.op('dve', lambda e: e.tensor_tensor(out=t0[:], in0=bv[:], in1=Winv[:], op=ALU.mult), r=[bv, Winv], w=[t0])
            P.op('dve', lambda e: e.tensor_tensor(out=t1[:], in0=k2[:], in1=Winv[:], op=ALU.mult), r=[k2, Winv], w=[t1])
            for hp in range(4):
                for qi, (srcb, dst) in enumerate(((Wexc, ART[:, hp, 0, :]), (Winc, ART[:, hp, 1, :]),
                                                  (t0, bT[:, hp, :]), (t1, kTt[:, hp, :]))):
                    p = pT[ntp % 2]
                    ntp += 1
                    dbuf = ART if qi < 2 else (bT if qi == 2 else kTt)
                    P.op('pe', lambda e, p=p, srcb=srcb, hp=hp: e.transpose(
                        out=p[:], in_=srcb[:, hp * 128:(hp + 1) * 128], identity=identf[:]), r=[srcb, identf], w=[p])
                    if qi % 2 == 0:
                        P.op('act', lambda e, p=p, dst=dst: e.activation(out=dst, in_=p[:], func=AF.Identity),
                             r=[p], w=[dbuf])
                    else:
                        P.op('dve', lambda e, p=p, dst=dst: e.tensor_copy(out=dst, in_=p[:]), r=[p], w=[dbuf])
            for hp in range(4):
                for hh in range(2):
                    h = hp * 2 + hh
                    pb = hh * 64
                    rhsAR = ART[pb:pb + 64, hp, :, :].rearrange("p a b -> p (a b)")
                    P.op('pe', lambda e, pb=pb, hp=hp, rhsAR=rhsAR: e.matmul(
                        pM[0][:, :], lhsT=bT[pb:pb + 64, hp, :], rhs=rhsAR,
                        start=True, stop=True), r=[bT, ART], w=[pM[0]])
                    P.op('dve', lambda e: e.tensor_tensor(out=Mb[:], in0=pM[0][:], in1=mask2[:], op=ALU.mult),
                         r=[pM[0], mask2], w=[Mb])
                    P.op('pe', lambda e, pb=pb, hp=hp, rhsAR=rhsAR: e.matmul(
                        pM[1][:, :], lhsT=kTt[pb:pb + 64, hp, :], rhs=rhsAR,
                        start=True, stop=True), r=[kTt, ART], w=[pM[1]])
                    P.op('dve', lambda e: e.tensor_tensor(out=Mk[:], in0=pM[1][:], in1=mask2[:], op=ALU.mult),
                         r=[pM[1], mask2], w=[Mk])
                    P.op('pe', lambda e, pb=pb, hp=hp: e.matmul(pQ[0][:], lhsT=ART[pb:pb + 64, hp, 0, :],
                                                               rhs=bT[pb:pb + 64, hp, :], start=True, stop=True),
                         r=[ART, bT], w=[pQ[0]])
                    P.op('dve', lambda e: e.tensor_tensor(out=NT[:], in0=pQ[0][:], in1=tril_s[:], op=ALU.mult),
                         r=[pQ[0], tril_s], w=[NT])
                    P.op('dve', lambda e: e.tensor_tensor(out=X[0][:], in0=Mb[:, 0:128], in1=identf[:], op=ALU.add),
                         r=[Mb, identf], w=[X[0]])
                    Pc, PcT = Mb, NT
                    xi = 0
                    for step in range(6):
                        Pn, PnT = Pm[step % 2], PmT[step % 2]
                        PcA = Pc[:, 0:128]
                        P.op('pe', lambda e, PcT=PcT, PcA=PcA: e.matmul(pQ[0][:], lhsT=PcT[:], rhs=PcA, start=True, stop=True),
                             r=[PcT, Pc], w=[pQ[0]])
                        P.op('pe', lambda e, PcT=PcT, PcA=PcA: e.matmul(pQ[1][:], lhsT=PcA, rhs=PcT[:], start=True, stop=True),
                             r=[PcT, Pc], w=[pQ[1]])
                        P.op('act', lambda e, Pn=Pn: e.activation(out=Pn[:], in_=pQ[0][:], func=AF.Identity), r=[pQ[0]], w=[Pn])
                        P.op('dve', lambda e, PnT=PnT: e.tensor_copy(out=PnT[:], in_=pQ[1][:]), r=[pQ[1]], w=[PnT])
                        Xc, Xn = X[xi], X[1 - xi]
                        P.op('pe', lambda e, Xc=Xc: e.matmul(pQ[0][:], lhsT=ident[:], rhs=Xc[:], start=True, stop=False),
                             r=[ident, Xc], w=[pQ[0]])
                        P.op('pe', lambda e, Xc=Xc, PnT=PnT: e.matmul(pQ[0][:], lhsT=PnT[:], rhs=Xc[:], start=False, stop=True),
                             r=[PnT, Xc], w=[pQ[0]])
                        P.op('act', lambda e, Xn=Xn: e.activation(out=Xn[:], in_=pQ[0][:], func=AF.Identity), r=[pQ[0]], w=[Xn])
                        xi = 1 - xi
                        Pc, PcT = Pn, PnT
                    Xf = X[xi]
                    tb = Tbf[hp]
                    P.op('pe', lambda e, pb=pb, hp=hp, tb=tb: e.matmul(pQ[1][:, 0:64], lhsT=ART[pb:pb + 64, hp, 0, :],
                                                                      rhs=tb[pb:pb + 64, :], start=True, stop=False),
                         r=[ART, tb], w=[pQ[1]])
                    P.op('pe', lambda e, h=h: e.matmul(pQ[1][:, 0:64], lhsT=Mk[:, 0:128], rhs=Vb[:, h * 64:(h + 1) * 64],
                                                       start=False, stop=True), r=[Mk, Vb], w=[pQ[1]])
                    P.op('act', lambda e: e.activation(out=Zs[:], in_=pQ[1][:, 0:64], func=AF.Identity), r=[pQ[1]], w=[Zs])
                    P.op('pe', lambda e, Xf=Xf: e.matmul(pQ[1][:, 64:128], lhsT=Xf[:], rhs=Zs[:], start=True, stop=True),
                         r=[Xf, Zs], w=[pQ[1]])
                    P.op('dve', lambda e, h=h: e.tensor_copy(out=UU[:, h * 64:(h + 1) * 64], in_=pQ[1][:, 64:128]),
                         r=[pQ[1]], w=[UU])
                    P.op('pe', lambda e, pb=pb, hp=hp, tb=tb: e.matmul(pQ[0][:, 0:64], lhsT=ART[pb:pb + 64, hp, 1, :],
                                                                      rhs=tb[pb:pb + 64, :], start=True, stop=False),
                         r=[ART, tb], w=[pQ[0]])
                    P.op('pe', lambda e, h=h: e.matmul(pQ[0][:, 0:64], lhsT=Mb[:, 128:256], rhs=UU[:, h * 64:(h + 1) * 64],
                                                       start=False, stop=False), r=[Mb, UU], w=[pQ[0]])
                    P.op('pe', lambda e, h=h: e.matmul(pQ[0][:, 0:64], lhsT=Mk[:, 128:256], rhs=Vb[:, h * 64:(h + 1) * 64],
                                                       start=False, stop=True), r=[Mk, Vb], w=[pQ[0]])
                    P.op('act', lambda e, h=h: e.activation(out=Ys[:, h * 64:(h + 1) * 64], in_=pQ[0][:, 0:64],
                                                            func=AF.Identity), r=[pQ[0]], w=[Ys])
                P.op('pe', lambda e, hp=hp: e.matmul(pQ[1][:], lhsT=Bh[:, hp * 128:(hp + 1) * 128],
                                                     rhs=UU[:, hp * 128:(hp + 1) * 128], start=True, stop=False),
                     r=[Bh, UU], w=[pQ[1]])
                P.op('pe', lambda e, hp=hp: e.matmul(pQ[1][:], lhsT=Kh[:, hp * 128:(hp + 1) * 128],
                                                     rhs=Vb[:, hp * 128:(hp + 1) * 128], start=False, stop=True),
                     r=[Kh, Vb], w=[pQ[1]])
                for hh in range(2):
                    pb = hh * 64
                    P.op('dve', lambda e, hp=hp, pb=pb: e.scalar_tensor_tensor(
                        out=Tst[hp][pb:pb + 64, :], in0=Tst[hp][pb:pb + 64, :], scalar=WLc[pb:pb + 64, hp:hp + 1],
                        in1=pQ[1][pb:pb + 64, pb:pb + 64], op0=ALU.mult, op1=ALU.add),
                         r=[Tst[hp], WLc, pQ[1]], w=[Tst[hp]])
                P.op('act', lambda e, hp=hp: e.activation(out=Tbf[hp][:], in_=Tst[hp][:], func=AF.Identity),
                     r=[Tst[hp]], w=[Tbf[hp]])
            P.op('dve', lambda e: e.tensor_reduce(out=sm[:, 8:16], in_=V3(Ys[:]), axis=AX.X, op=ALU.add), r=[Ys], w=[sm])
            P.op('dve', lambda e: e.tensor_scalar(out=sm[:, 8:16], in0=sm[:, 8:16], scalar1=1.0 / 64, scalar2=None,
                                                  op0=ALU.mult), r=[sm], w=[sm])
            P.op('dve', lambda e: e.tensor_tensor(out=V3(Ys[:]), in0=V3(Ys[:]), in1=B3(sm[:, 8:16]), op=ALU.subtract),
                 r=[Ys, sm], w=[Ys])
            P.op('dve', lambda e: e.tensor_tensor(out=t0[:], in0=Ys[:], in1=Ys[:], op=ALU.mult), r=[Ys], w=[t0])
            P.op('dve', lambda e: e.tensor_reduce(out=sm[:, 16:24], in_=V3(t0[:]), axis=AX.X, op=ALU.add), r=[t0], w=[sm])
            P.op('act', lambda e: e.activation(out=sm[:, 16:24], in_=sm[:, 16:24], func=AF.Sqrt, scale=1.0 / 64,
                                               bias=64e-5), r=[sm], w=[sm])
            P.op('dve', lambda e: e.reciprocal(out=sm[:, 16:24], in_=sm[:, 16:24]), r=[sm], w=[sm])
            P.op('dve', lambda e: e.tensor_tensor(out=V3(Ys[:]), in0=V3(Ys[:]), in1=B3(sm[:, 16:24]), op=ALU.mult),
                 r=[Ys, sm], w=[Ys])
            P.op('dve', lambda e: e.tensor_tensor(out=Ys[:], in0=Ys[:], in1=lnw, op=ALU.mult), r=[Ys, vec], w=[Ys])
            P.op('dve', lambda e: e.tensor_tensor(out=Ys[:], in0=Ys[:], in1=lnb, op=ALU.add), r=[Ys, vec], w=[Ys])
            P.op('dve', lambda e: e.tensor_tensor(out=t0[:], in0=k2[:], in1=r_, op=ALU.mult), r=[k2, xs], w=[t0])
            P.op('dve', lambda e: e.tensor_tensor(out=t0[:], in0=t0[:], in1=rkb, op=ALU.mult), r=[t0, vec], w=[t0])
            P.op('dve', lambda e: e.tensor_reduce(out=sm[:, 24:32], in_=V3(t0[:]), axis=AX.X, op=ALU.add), r=[t0], w=[sm])
            P.op('dve', lambda e: e.tensor_tensor(out=V3(t0[:]), in0=V3(v_), in1=B3(sm[:, 24:32]), op=ALU.mult),
                 r=[xs, sm], w=[t0])
            P.op('dve', lambda e: e.tensor_tensor(out=Ys[:], in0=Ys[:], in1=t0[:], op=ALU.add), r=[Ys, t0], w=[Ys])
            P.op('dve', lambda e, c=c: e.tensor_tensor(out=yb[c % 2][:], in0=Ys[:], in1=gg[:], op=ALU.mult),
                 r=[Ys, gg], w=[yb[c % 2]])
            P.dma('sp', y_out[c * 128:(c + 1) * 128, 1536:2048], yb[c % 2][:], r=[yb[c % 2]],
                  w=[P.tok('y', 'rwkv', c)], key=yb[c % 2].name)


D = 4096
KC = 32
NCOL = 5988
C_M = 0
C_L = 1540
C_R = 2628
C_W = 4164
NW = 1824
PI = math.pi
LDEC = -math.exp(-0.5)


def build_p2(S, parts=('A', 'ret', 'mlstm', 'mla', 'rwkv')):
    nc = bass.Bass("TRN2", target_bir_lowering=False)
    P = Prog(nc)
    NCH = S // 128
    din = lambda n, shp, dt=F32: nc.dram_tensor(n, list(shp), dt, kind="ExternalInput").ap()
    hT = din("hT", [D, S], BF16)
    w_in = din("w_in", [D, NCOL])
    pos = din("pos", [128, S // 128], I32)
    mbias = din("mbias", [1, 4])
    lgam = din("lgam", [1, 2])
    qn_c = din("qn_c", [128, 4])
    kvn_c = din("kvn_c", [128, 4])
    w_uq = din("w_uq", [512, 768])
    w_ukv = din("w_ukv", [512, 1024])
    rw_mu = din("rw_mu", [1, NW])
    rw_vec = din("rw_vec", [7, 512])
    rw_w2 = din("rw_w2", [64, 512])
    rw_a2 = din("rw_a2", [64, 512])
    rw_g2 = din("rw_g2", [160, 512])
    y_out = nc.dram_tensor("y_out", [S, 2048], BF16, kind="ExternalOutput").ap()
    proj = nc.dram_tensor("proj", [S, NCOL], F32, kind="Internal").ap()

    def mk_consts():
        c = {}
        c['identf'] = P.sb("identf", [128, 128], F32)
        c['ident'] = P.sb("ident", [128, 128], BF16)
        c['triu'] = P.sb("triu", [128, 128], F32)
        c['trius'] = P.sb("trius", [128, 128], F32)
        c['tril_s'] = P.sb("tril_s", [128, 128], F32)
        c['ones'] = P.sb("ones", [128, 128], F32)
        idf, tu, tus, tls, on = c['identf'], c['triu'], c['trius'], c['tril_s'], c['ones']
        P.op('pool', lambda e: e.memset(idf[:], 0.0), w=[idf])
        P.op('pool', lambda e: e.affine_select(out=idf[:], in_=idf[:], pattern=[[-1, 128]],
                                               compare_op=ALU.not_equal, fill=1.0, base=0,
                                               channel_multiplier=1), r=[idf], w=[idf])
        P.op('pool', lambda e: e.memset(on[:], 1.0), w=[on])
        P.op('pool', lambda e: e.affine_select(out=tu[:], in_=on[:], pattern=[[1, 128]],
                                               compare_op=ALU.is_ge, fill=0.0, base=0,
                                               channel_multiplier=-1), r=[on], w=[tu])
        P.op('pool', lambda e: e.affine_select(out=tus[:], in_=on[:], pattern=[[1, 128]],
                                               compare_op=ALU.is_gt, fill=0.0, base=0,
                                               channel_multiplier=-1), r=[on], w=[tus])
        P.op('pool', lambda e: e.affine_select(out=tls[:], in_=on[:], pattern=[[-1, 128]],
                                               compare_op=ALU.is_gt, fill=0.0, base=0,
                                               channel_multiplier=1), r=[on], w=[tls])
        P.op('dve', lambda e: e.tensor_copy(out=c['ident'][:], in_=idf[:]), r=[idf], w=[c['ident']])
        return c

    def rope_tables(nfreq, dfull, name):
        cs = P.sb(name + "_cos", [128, NCH, nfreq], F32)
        sn = P.sb(name + "_sin", [128, NCH, nfreq], F32)
        with P.scope():
            pi_ = P.sb("pos_i", [128, NCH], I32)
            pf = P.sb("pos_f", [128, NCH], F32)
            fr = P.sb("fr", [128, nfreq], F32)
            fi = P.sb("fi", [128, nfreq], I32)
            ang = P.sb("ang", [128, NCH, nfreq], F32)
            ki = P.sb("ki", [128, NCH, nfreq], I32)
            kf = P.sb("kf", [128, NCH, nfreq], F32)
            m = P.sb("mm", [128, NCH, nfreq], F32)
            P.dma('sp', pi_[:], pos[:, :], w=[pi_], key='pos_i')
            P.op('dve', lambda e: e.tensor_copy(out=pf[:], in_=pi_[:]), r=[pi_], w=[pf])
            P.op('pool', lambda e: e.iota(fi[:], pattern=[[1, nfreq]], base=0, channel_multiplier=0),
                 w=[fi])
            P.op('dve', lambda e: e.tensor_copy(out=fr[:], in_=fi[:]), r=[fi], w=[fr])
            P.op('act', lambda e: e.activation(out=fr[:], in_=fr[:], func=AF.Exp,
                                               scale=-2.0 / dfull * math.log(10000.0)),
                 r=[fr], w=[fr])
            for c in range(NCH):
                P.op('dve', lambda e, c=c: e.tensor_scalar(out=ang[:, c, :], in0=fr[:],
                                                           scalar1=pf[:, c:c + 1], scalar2=None,
                                                           op0=ALU.mult), r=[fr, pf], w=[ang])

            def wrap_sin(dst, shift):
                P.op('dve', lambda e: e.tensor_scalar(out=kf[:], in0=ang[:], scalar1=shift,
                                                      scalar2=1.0 / (2 * PI), op0=ALU.add,
                                                      op1=ALU.mult), r=[ang], w=[kf])
                P.op('dve', lambda e: e.tensor_copy(out=ki[:], in_=kf[:]), r=[kf], w=[ki])
                P.op('dve', lambda e: e.tensor_copy(out=kf[:], in_=ki[:]), r=[ki], w=[kf])
                P.op('dve', lambda e: e.scalar_tensor_tensor(out=m[:], in0=kf[:], scalar=-2 * PI,
                                                             in1=ang[:], op0=ALU.mult, op1=ALU.add),
                     r=[kf, ang], w=[m])
                P.op('dve', lambda e: e.tensor_scalar(out=m[:], in0=m[:], scalar1=shift,
                                                      scalar2=-PI, op0=ALU.add, op1=ALU.max),
                     r=[m], w=[m])
                P.op('dve', lambda e: e.tensor_scalar(out=m[:], in0=m[:], scalar1=PI, scalar2=None,
                                                      op0=ALU.min), r=[m], w=[m])
                P.op('act', lambda e: e.activation(out=dst[:], in_=m[:], func=AF.Sin), r=[m], w=[dst])
            wrap_sin(sn, 0.0)
            wrap_sin(cs, PI / 2)
        return cs, sn

    def rope_tm(src, dst, ngrp, half, cs, sn, c, tmp):
        sv = src.rearrange("p (g t h) -> p g t h", g=ngrp, t=2)
        dv = dst.rearrange("p (g t h) -> p g t h", g=ngrp, t=2)
        cb = cs[:, c:c + 1, :].to_broadcast([128, ngrp, half])
        sb_ = sn[:, c:c + 1, :].to_broadcast([128, ngrp, half])
        x1, x2 = sv[:, :, 0, :], sv[:, :, 1, :]
        t1 = tmp[:, 0:ngrp * half].rearrange("p (g h) -> p g h", g=ngrp)
        t2 = tmp[:, ngrp * half:2 * ngrp * half].rearrange("p (g h) -> p g h", g=ngrp)
        return [
            lambda e: e.tensor_tensor(out=t1, in0=x1, in1=cb, op=ALU.mult),
            lambda e: e.tensor_tensor(out=t2, in0=x2, in1=sb_, op=ALU.mult),
            lambda e: e.tensor_tensor(out=dv[:, :, 0, :], in0=t1, in1=t2, op=ALU.subtract),
            lambda e: e.tensor_tensor(out=t1, in0=x1, in1=sb_, op=ALU.mult),
            lambda e: e.tensor_tensor(out=t2, in0=x2, in1=cb, op=ALU.mult),
            lambda e: e.tensor_tensor(out=dv[:, :, 1, :], in0=t1, in1=t2, op=ALU.add),
        ]

    if 'A' in parts:
        with P.scope():
            aT = P.sb("aT", [128, KC, 512], BF16)
            wt = [P.sb(f"wt{i}", [128, KC, 512], BF16) for i in range(2)]
            stg = [P.sb(f"stg{i}", [128, 512], F32) for i in range(3)]
            pp = [P.ps(f"pp{i}", [128, 512]) for i in range(4)]
            hv = hT.rearrange("(k p) n -> p k n", p=128)
            wv = w_in.rearrange("(k p) n -> p k n", p=128)
            nw = ns = npp = 0
            for t0 in range(0, S, 512):
                TT = min(512, S - t0)
                P.dma('sp', aT[:, :, 0:TT], hv[:, :, t0:t0 + TT], w=[aT], key='aT')
                for n0 in range(0, NCOL, 512):
                    nn = min(512, NCOL - n0)
                    w = wt[nw % 2]
                    nw += 1
                    for q in range(4):
                        P.dma('pool', w[:, q * 8:(q + 1) * 8, 0:nn], wv[:, q * 8:(q + 1) * 8, n0:n0 + nn],
                              w=[w], key=w.name)
                    for c in range(TT // 128):
                        p = pp[npp % 4]
                        npp += 1
                        for k in range(KC):
                            P.op('pe', lambda e, p=p, k=k, c=c, w=w, nn=nn: e.matmul(
                                p[:, 0:nn], lhsT=aT[:, k, c * 128:(c + 1) * 128], rhs=w[:, k, 0:nn],
                                start=(k == 0), stop=(k == KC - 1)), r=[aT, w], w=[p])
                        s = stg[ns % 3]
                        ns += 1
                        P.op('act', lambda e, p=p, s=s, nn=nn: e.activation(
                            out=s[:, 0:nn], in_=p[:, 0:nn], func=AF.Identity), r=[p], w=[s])
                        P.dma('sp', proj[t0 + c * 128:t0 + (c + 1) * 128, n0:n0 + nn], s[:, 0:nn],
                              r=[s], w=[P.tok('proj', (t0 // 128) + c, n0)], key=s.name)

    def proj_toks(c):
        return [P.tok('proj', c, n0) for n0 in range(0, NCOL, 512)] if 'A' in parts else []

    def linattn_phase(kind):
        is_m = kind == 'mlstm'
        col0 = C_M if is_m else C_R
        ncols = 1540 if is_m else 1536
        ycol = 0 if is_m else 1024
        with P.scope():
            cst = mk_consts()
            identf, triu, ones = cst['identf'], cst['triu'], cst['ones']
            pr = [P.sb(f"pr{i}", [128, ncols], F32) for i in range(2)]
            qk = P.sb("qk", [128, 512], F32)
            tmp = P.sb("tmp", [128, 512], F32)
            qT = [P.sb(f"qT{h}", [128, 128], BF16) for h in range(2)]
            qsT = [P.sb(f"qsT{h}", [128, 128], BF16) for h in range(2)]
            kT = [P.sb(f"kT{h}", [128, 128], BF16) for h in range(2)]
            kw = [P.sb(f"kw{h}", [128, 128], BF16) for h in range(2)]
            Vb = [P.sb(f"Vb{h}", [128, 257], BF16) for h in range(2)]
            PT = [P.sb(f"PT{h}", [128, 128], BF16) for h in range(2)]
            Cst = [P.sb(f"Cst{h}", [128, 257], F32) for h in range(2)]
            Cbf = [P.sb(f"Cbf{h}", [128, 257], BF16) for h in range(2)]
            WT = [P.sb(f"WT{h}", [128, 128], F32) for h in range(2)]
            qdec = [P.sb(f"qdec{h}", [128, 128], F32) for h in range(2)]
            sm = P.sb("sm", [128, 32], F32)
            yn = P.sb("yn", [128, 256], F32)
            gate = P.sb("gate", [128, 512], F32)
            yb = [P.sb(f"yb{i}", [128, 512], BF16) for i in range(2)]
            junk = P.sb("junk", [128, 256], BF16)
            ptr = [P.ps(f"ptr{i}", [128, 128]) for i in range(2)]
            pS = [P.ps(f"pS{i}", [128, 128]) for i in range(2)]
            pO = [P.ps(f"pO{i}", [128, 257]) for i in range(2)]
            pC = P.ps("pC", [128, 257])
            pB = P.ps("pB", [128, 128])
            for h in range(2):
                P.op('dve', lambda e, h=h: e.memset(Cst[h][:], 0.0), w=[Cst[h]])
                P.op('dve', lambda e, h=h: e.memset(Cbf[h][:], 0.0), w=[Cbf[h]])
                P.op('dve', lambda e, h=h: e.memset(Vb[h][:, 256:257], 1.0), w=[Vb[h]])
            if is_m:
                mb = P.sb("mb", [128, 4], F32)
                P.dma('sp', mb[:], mbias.partition_broadcast(128), w=[mb], key='mb')
                gts = P.sb("gts", [128, 8], F32)
                lfb = P.sb("lfb", [128, 128], F32)
                brs = [P.sb(f"brs{h}", [128, 128], F32) for h in range(2)]
                bcol = P.sb("bcol", [128, 2], F32)
            else:
                cs, sn = rope_tables(64, 128, "r")
                lg = P.sb("lg", [128, 2], F32)
                P.dma('sp', lg[:], lgam.partition_broadcast(128), w=[lg], key='lg')
                io = P.sb("io", [128, 128], F32)
                ioi = P.sb("ioi", [128, 128], I32)
                kdec = P.sb("kdec", [128, 2], F32)
                cdec = P.sb("cdec", [128, 2], F32)
                P.op('pool', lambda e: e.iota(ioi[:], pattern=[[1, 128]], base=0, channel_multiplier=-1),
                     w=[ioi])
                P.op('dve', lambda e: e.tensor_copy(out=io[:], in_=ioi[:]), r=[ioi], w=[io])
                for h in range(2):
                    P.op('act', lambda e, h=h: e.activation(out=WT[h][:], in_=io[:], func=AF.Exp,
                                                            scale=lg[:, h:h + 1]), r=[io, lg], w=[WT[h]])
                    P.op('dve', lambda e, h=h: e.tensor_tensor(out=WT[h][:], in0=WT[h][:], in1=triu[:],
                                                              op=ALU.mult), r=[WT[h], triu], w=[WT[h]])
                P.op('pool', lambda e: e.iota(ioi[:], pattern=[[1, 128]], base=1, channel_multiplier=0),
                     r=[io], w=[ioi])
                P.op('dve', lambda e: e.tensor_copy(out=io[:], in_=ioi[:]), r=[ioi], w=[io])
                for h in range(2):
                    P.op('act', lambda e, h=h: e.activation(out=qdec[h][:], in_=io[:], func=AF.Exp,
                                                            scale=lg[:, h:h + 1]), r=[io, lg], w=[qdec[h]])
                P.op('pool', lambda e: e.iota(ioi[:, 0:1], pattern=[[0, 1]], base=127, channel_multiplier=-1),
                     r=[io], w=[ioi])
                P.op('dve', lambda e: e.tensor_copy(out=io[:, 0:1], in_=ioi[:, 0:1]), r=[ioi], w=[io])
                for h in range(2):
                    P.op('act', lambda e, h=h: e.activation(out=kdec[:, h:h + 1], in_=io[:, 0:1], func=AF.Exp,
                                                            scale=lg[:, h:h + 1]), r=[io, lg], w=[kdec])
                    P.op('act', lambda e, h=h: e.activation(out=cdec[:, h:h + 1], in_=lg[:, h:h + 1],
                                                            func=AF.Exp, scale=128.0), r=[lg], w=[cdec])
            ntr = 0
            STOP = 99
            for c in range(NCH if STOP > 0 else 0):
                p_ = pr[c % 2]
                P.dma('sp', p_[:], proj[c * 128:(c + 1) * 128, col0:col0 + ncols], r=proj_toks(c),
                      w=[p_], key=p_.name)
                if is_m:
                    src = p_
                    qscale = 1.0
                else:
                    for f in rope_tm(p_[:, 0:512], qk[:], 4, 64, cs, sn, c, tmp):
                        P.op('dve', f, r=[p_, cs, sn, tmp, qk], w=[tmp, qk])
                    src = qk
                    qscale = 128.0 ** -0.5
                if STOP == 1:
                    continue
                if is_m:
                    P.op('dve', lambda e, p_=p_: e.tensor_tensor(out=gts[:, 0:4], in0=p_[:, 1536:1540],
                                                                 in1=mb[:], op=ALU.add), r=[p_, mb], w=[gts])
                    P.op('dve', lambda e: e.tensor_scalar(out=gts[:, 0:2], in0=gts[:, 0:2],
                                                          scalar1=math.log(128.0 ** -0.5), scalar2=None,
                                                          op0=ALU.add), r=[gts], w=[gts])
                    P.op('act', lambda e: e.activation(out=gts[:, 4:6], in_=gts[:, 2:4], func=AF.Exp,
                                                       scale=-1.0), r=[gts], w=[gts])
                    P.op('act', lambda e: e.activation(out=gts[:, 4:6], in_=gts[:, 4:6], func=AF.Ln,
                                                       bias=1.0), r=[gts], w=[gts])
                    P.op('dve', lambda e: e.tensor_scalar(out=gts[:, 2:4], in0=gts[:, 4:6], scalar1=-1.0,
                                                          scalar2=None, op0=ALU.mult), r=[gts], w=[gts])
                    P.op('pe', lambda e: e.matmul(pB[:, 0:2], lhsT=triu[:], rhs=gts[:, 2:4], start=True,
                                                  stop=True), r=[triu, gts], w=[pB])
                    P.op('dve', lambda e: e.tensor_copy(out=bcol[:], in_=pB[:, 0:2]), r=[pB], w=[bcol])
                    P.op('dve', lambda e: e.tensor_tensor(out=gts[:, 6:8], in0=gts[:, 0:2], in1=bcol[:],
                                                          op=ALU.subtract), r=[gts, bcol], w=[gts])
                    for h in range(2):
                        P.op('dve', lambda e, h=h: e.tensor_scalar(out=lfb[:], in0=ones[:],
                                                                   scalar1=gts[:, 2 + h:3 + h], scalar2=None,
                                                                   op0=ALU.mult), r=[ones, gts], w=[lfb])
                        P.op('pe', lambda e: e.matmul(pB[:], lhsT=lfb[:], rhs=triu[:], start=True, stop=True),
                             r=[lfb, triu], w=[pB])
                        P.op('act', lambda e, h=h: e.activation(out=brs[h][:], in_=pB[:], func=AF.Identity),
                             r=[pB], w=[brs[h]])
                        P.op('act', lambda e, h=h: e.activation(out=WT[h][:], in_=brs[h][:], func=AF.Exp,
                                                                bias=gts[:, 6 + h:7 + h]), r=[brs[h], gts],
                             w=[WT[h]])
                        P.op('dve', lambda e, h=h: e.tensor_tensor(out=WT[h][:], in0=WT[h][:], in1=triu[:],
                                                                  op=ALU.mult), r=[WT[h], triu], w=[WT[h]])
                        P.op('act', lambda e, h=h: e.activation(out=qdec[h][:], in_=brs[h][:], func=AF.Exp),
                             r=[brs[h]], w=[qdec[h]])
                        P.op('act', lambda e, h=h: e.activation(out=sm[:, h:h + 1], in_=gts[:, 6 + h:7 + h],
                                                                func=AF.Exp, bias=brs[h][:, 127:128]),
                             r=[gts, brs[h]], w=[sm])
                        P.op('act', lambda e, h=h: e.activation(out=sm[:, 2 + h:3 + h], in_=brs[h][:, 127:128],
                                                                func=AF.Exp), r=[brs[h]], w=[sm])
                for h in range(2):
                    voff = 512 + h * 256
                    for which, dst in ((0, qT[h]), (1, kT[h])):
                        p = ptr[ntr % 2]
                        ntr += 1
                        off = which * 256 + h * 128
                        P.op('pe', lambda e, p=p, off=off, src=src: e.transpose(
                            out=p[:], in_=src[:, off:off + 128], identity=identf[:]), r=[src, identf], w=[p])
                        if which == 0:
                            P.op('act', lambda e, p=p, dst=dst: e.activation(out=dst[:], in_=p[:], func=AF.Identity,
                                                                            scale=qscale), r=[p], w=[dst])
                            P.op('dve', lambda e, h=h: e.tensor_tensor(out=qsT[h][:], in0=qT[h][:], in1=qdec[h][:],
                                                                      op=ALU.mult), r=[qT[h], qdec[h]], w=[qsT[h]])
                        else:
                            P.op('act', lambda e, p=p, dst=dst: e.activation(out=dst[:], in_=p[:], func=AF.Identity),
                                 r=[p], w=[dst])
                    koff = 256 + h * 128
                    if is_m:
                        P.op('dve', lambda e, h=h, koff=koff, src=src: e.tensor_scalar(
                            out=kw[h][:], in0=src[:, koff:koff + 128], scalar1=sm[:, h:h + 1], scalar2=None,
                            op0=ALU.mult), r=[src, sm], w=[kw[h]])
                    else:
                        P.op('dve', lambda e, h=h, koff=koff, src=src: e.tensor_scalar(
                            out=kw[h][:], in0=src[:, koff:koff + 128], scalar1=kdec[:, h:h + 1], scalar2=None,
                            op0=ALU.mult), r=[src, kdec], w=[kw[h]])
                    P.op('act', lambda e, h=h, voff=voff, p_=p_: e.activation(
                        out=Vb[h][:, 0:256], in_=p_[:, voff:voff + 256], func=AF.Identity), r=[p_], w=[Vb[h]])
                    if STOP == 2:
                        continue
                    ps = pS[h]
                    P.op('pe', lambda e, h=h, ps=ps: e.matmul(ps[:], lhsT=kT[h][:], rhs=qT[h][:], start=True,
                                                             stop=True), r=[kT[h], qT[h]], w=[ps])
                    P.op('dve', lambda e, h=h, ps=ps: e.tensor_tensor(out=PT[h][:], in0=ps[:], in1=WT[h][:],
                                                                     op=ALU.mult), r=[ps, WT[h]], w=[PT[h]])
                    if STOP == 3:
                        continue
                    po = pO[h]
                    P.op('pe', lambda e, h=h, po=po: e.matmul(po[:], lhsT=PT[h][:], rhs=Vb[h][:], start=True,
                                                             stop=False), r=[PT[h], Vb[h]], w=[po])
                    P.op('pe', lambda e, h=h, po=po: e.matmul(po[:], lhsT=qsT[h][:], rhs=Cbf[h][:], start=False,
                                                             stop=True), r=[qsT[h], Cbf[h]], w=[po])
                    if STOP == 4:
                        continue
                    P.op('pe', lambda e, h=h: e.matmul(pC[:], lhsT=kw[h][:], rhs=Vb[h][:], start=True, stop=True),
                         r=[kw[h], Vb[h]], w=[pC])
                    dsc = sm[:, 2 + h:3 + h] if is_m else cdec[:, h:h + 1]
                    P.op('dve', lambda e, h=h, dsc=dsc: e.scalar_tensor_tensor(
                        out=Cst[h][:], in0=Cst[h][:], scalar=dsc, in1=pC[:], op0=ALU.mult, op1=ALU.add),
                         r=[Cst[h], pC, sm] + ([] if is_m else [cdec]), w=[Cst[h]])
                    P.op('act', lambda e, h=h: e.activation(out=Cbf[h][:], in_=Cst[h][:], func=AF.Identity),
                         r=[Cst[h]], w=[Cbf[h]])
                    if STOP == 5:
                        continue
                    ybuf = yb[c % 2]
                    b0 = 8 + h * 8
                    if is_m:
                        P.op('act', lambda e, po=po, b0=b0: e.activation(out=sm[:, b0:b0 + 1], in_=po[:, 256:257],
                                                                        func=AF.Abs), r=[po], w=[sm])
                        P.op('dve', lambda e, b0=b0: e.tensor_scalar(out=sm[:, b0:b0 + 1], in0=sm[:, b0:b0 + 1],
                                                                     scalar1=1.0, scalar2=None, op0=ALU.max),
                             r=[sm], w=[sm])
                        P.op('dve', lambda e, b0=b0: e.reciprocal(out=sm[:, b0:b0 + 1], in_=sm[:, b0:b0 + 1]),
                             r=[sm], w=[sm])
                        P.op('act', lambda e, po=po, b0=b0: e.activation(
                            out=junk[:], in_=po[:, 0:256], func=AF.Square, scale=sm[:, b0:b0 + 1],
                            accum_out=sm[:, b0 + 1:b0 + 2]), r=[po, sm], w=[junk, sm])
                        P.op('act', lambda e, b0=b0: e.activation(out=sm[:, b0 + 1:b0 + 2], in_=sm[:, b0 + 1:b0 + 2],
                                                                  func=AF.Sqrt, scale=1.0 / 256, bias=1e-6),
                             r=[sm], w=[sm])
                        P.op('dve', lambda e, b0=b0: e.reciprocal(out=sm[:, b0 + 1:b0 + 2], in_=sm[:, b0 + 1:b0 + 2]),
                             r=[sm], w=[sm])
                        P.op('dve', lambda e, b0=b0: e.tensor_tensor(out=sm[:, b0 + 2:b0 + 3], in0=sm[:, b0:b0 + 1],
                                                                     in1=sm[:, b0 + 1:b0 + 2], op=ALU.mult),
                             r=[sm], w=[sm])
                        P.op('act', lambda e, h=h, p_=p_: e.activation(
                            out=gate[:, h * 256:(h + 1) * 256], in_=p_[:, 1024 + h * 256:1280 + h * 256],
                            func=AF.Sigmoid), r=[p_], w=[gate])
                        P.op('dve', lambda e, h=h, po=po, b0=b0, ybuf=ybuf: e.scalar_tensor_tensor(
                            out=ybuf[:, h * 256:(h + 1) * 256], in0=po[:, 0:256], scalar=sm[:, b0 + 2:b0 + 3],
                            in1=gate[:, h * 256:(h + 1) * 256], op0=ALU.mult, op1=ALU.mult),
                             r=[po, sm, gate], w=[ybuf])
                    else:
                        P.op('act', lambda e, po=po, b0=b0: e.activation(out=junk[:], in_=po[:, 0:256], func=AF.Identity,
                                                                        accum_out=sm[:, b0:b0 + 1]),
                             r=[po], w=[junk, sm])
                        P.op('act', lambda e, po=po, b0=b0: e.activation(out=junk[:], in_=po[:, 0:256], func=AF.Square,
                                                                        accum_out=sm[:, b0 + 1:b0 + 2]),
                             r=[po], w=[junk, sm])
                        P.op('dve', lambda e, b0=b0: e.tensor_scalar(out=sm[:, b0:b0 + 1], in0=sm[:, b0:b0 + 1],
                                                                     scalar1=1.0 / 256, scalar2=None, op0=ALU.mult),
                             r=[sm], w=[sm])
                        P.op('dve', lambda e, b0=b0: e.tensor_tensor(out=sm[:, b0 + 2:b0 + 3], in0=sm[:, b0:b0 + 1],
                                                                     in1=sm[:, b0:b0 + 1], op=ALU.mult),
                             r=[sm], w=[sm])
                        P.op('dve', lambda e, b0=b0: e.scalar_tensor_tensor(
                            out=sm[:, b0 + 1:b0 + 2], in0=sm[:, b0 + 1:b0 + 2], scalar=1.0 / 256,
                            in1=sm[:, b0 + 2:b0 + 3], op0=ALU.mult, op1=ALU.subtract), r=[sm], w=[sm])
                        P.op('act', lambda e, b0=b0: e.activation(out=sm[:, b0 + 1:b0 + 2], in_=sm[:, b0 + 1:b0 + 2],
                                                                  func=AF.Sqrt, bias=1e-6), r=[sm], w=[sm])
                        P.op('dve', lambda e, b0=b0: e.reciprocal(out=sm[:, b0 + 1:b0 + 2], in_=sm[:, b0 + 1:b0 + 2]),
                             r=[sm], w=[sm])
                        P.op('dve', lambda e, b0=b0: e.scalar_tensor_tensor(
                            out=sm[:, b0 + 2:b0 + 3], in0=sm[:, b0:b0 + 1], scalar=-1.0, in1=sm[:, b0 + 1:b0 + 2],
                            op0=ALU.mult, op1=ALU.mult), r=[sm], w=[sm])
                        P.op('act', lambda e, po=po, b0=b0: e.activation(
                            out=yn[:], in_=po[:, 0:256], func=AF.Identity, scale=sm[:, b0 + 1:b0 + 2],
                            bias=sm[:, b0 + 2:b0 + 3]), r=[po, sm], w=[yn])
                        P.op('act', lambda e, h=h, p_=p_: e.activation(
                            out=gate[:, h * 256:(h + 1) * 256], in_=p_[:, 1024 + h * 256:1280 + h * 256],
                            func=AF.Silu), r=[p_], w=[gate])
                        P.op('dve', lambda e, h=h, ybuf=ybuf: e.tensor_tensor(
                            out=ybuf[:, h * 256:(h + 1) * 256], in0=yn[:], in1=gate[:, h * 256:(h + 1) * 256],
                            op=ALU.mult), r=[yn, gate], w=[ybuf])
                P.dma('sp', y_out[c * 128:(c + 1) * 128, ycol:ycol + 512], yb[c % 2][:], r=[yb[c % 2]],
                      w=[P.tok('y', kind, c)], key=yb[c % 2].name)

    if 'ret' in parts:
        linattn_phase('ret')
    if 'mlstm' in parts:
        linattn_phase('mlstm')
    if 'mla' in parts:
        mla_phase(P, locals())
    if 'rwkv' in parts:
        rwkv_phase(P, locals())

    P.wait_all('sp', [v for k, v in P.tokens.items() if k[0] == 'y'])
    P.emit()
    return nc


def col(g):
    return np.ascontiguousarray(np.asarray(g, np.float32).reshape(-1, 128).T)

def p2_inputs(I, l, hh, hT_b, pos_b):
    S = hT_b.shape[1]
    w = I['w_in'][l]
    def sl(base, off, n): return w[:, base + off: base + off + n]
    m0, l0, r0, k0 = 0, 3080, 4168, 7240
    cols = [sl(m0, 2*hh*128, 256), sl(m0, 512 + 2*hh*128, 256), sl(m0, 1024 + 2*hh*256, 512),
            sl(m0, 2048 + 2*hh*256, 512), sl(m0, 3072 + 2*hh, 2), sl(m0, 3076 + 2*hh, 2),
            sl(l0, 0, 1088),
            sl(r0, 2*hh*128, 256), sl(r0, 512 + 2*hh*128, 256), sl(r0, 1024 + 2*hh*256, 512),
            sl(r0, 2048 + 2*hh*256, 512),
            sl(k0, hh*512, 512), sl(k0, 1024 + hh*512, 512), sl(k0, 2048 + hh*512, 512),
            sl(k0, 3072, 64), sl(k0, 3136, 64), sl(k0, 3200, 160)]
    w_c = np.ascontiguousarray(np.concatenate(cols, axis=1))
    mu = I['rwkv_mu'][l]
    mu_c = np.concatenate([mu[hh*512:hh*512+512], mu[1024+hh*512:1024+hh*512+512],
                           mu[2048+hh*512:2048+hh*512+512], mu[3072:3360]])[None, :]
    hs = slice(hh*512, hh*512+512)
    vec = np.stack([I['rwkv_w0'][l][hs], I['rwkv_a0'][l][hs], I['rwkv_k_k'][l][hs], I['rwkv_k_a'][l][hs],
                    I['rwkv_ln_w'][l][hs], I['rwkv_ln_b'][l][hs], I['rwkv_r_k'][l].reshape(-1)[hs]])
    Hh = np.arange(2*hh, 2*hh+2, dtype=np.float32)
    return {
        "hT": hT_b, "w_in": w_c,
        "pos": np.ascontiguousarray(np.asarray(pos_b, np.int32).reshape(S // 128, 128).T),
        "mbias": np.concatenate([I['mlstm_i_bias'][l][2*hh:2*hh+2], I['mlstm_f_bias'][l][2*hh:2*hh+2]])[None, :].astype(np.float32),
        "lgam": np.log1p(-np.exp2(-5.0 - np.array([[2*hh, 2*hh+1]], np.float64))).astype(np.float32),
        "qn_c": col(I['mla_q_norm'][l]), "kvn_c": col(I['mla_kv_norm'][l]),
        "w_uq": np.ascontiguousarray(I['mla_w_uq'][l][:, 4*hh*192:(4*hh+4)*192]),
        "w_ukv": np.ascontiguousarray(I['mla_w_ukv'][l][:, 4*hh*256:(4*hh+4)*256]),
        "rw_mu": np.ascontiguousarray(mu_c.astype(np.float32)), "rw_vec": np.ascontiguousarray(vec.astype(np.float32)),
        "rw_w2": np.ascontiguousarray(I['rwkv_w2'][l][:, hs]), "rw_a2": np.ascontiguousarray(I['rwkv_a2'][l][:, hs]),
        "rw_g2": np.ascontiguousarray(I['rwkv_g2'][l][:, hs]),
    }


_PROGS = {}


def _prog(name):
    if name not in _PROGS:
        if name == 'p0':
            _PROGS[name] = build_p3(2048, False, False)
        elif name == 'p2':
            _PROGS[name] = build_p2(4096)
        elif name == 'p3':
            _PROGS[name] = build_p3(2048, True, False)
        else:
            _PROGS[name] = build_p3(2048, True, True)
    return _PROGS[name]


def kernel(**inputs):
    I = {k: np.asarray(v) for k, v in inputs.items()}
    NCORE = 8
    x = np.ascontiguousarray(I['x'].reshape(16384, 4096).astype(np.float32, copy=False))
    pos = I['positions']
    cores = list(range(NCORE))
    xs = [x[c * 2048:(c + 1) * 2048] for c in cores]
    res = run_bass_kernel_spmd(_prog('p0'), [{"x_in": xs[c], "gnext": col(I['attn_norm'][0])} for c in cores],
                               core_ids=cores)
    hT = [res.results[c]["hT_out"] for c in cores]
    out = None
    for l in range(4):
        maps = []
        for c in cores:
            b, hh = c // 2, c % 2
            hT_b = np.ascontiguousarray(np.concatenate([hT[2 * b], hT[2 * b + 1]], axis=1))
            maps.append(p2_inputs(I, l, hh, hT_b, pos[b]))
        res = run_bass_kernel_spmd(_prog('p2'), maps, core_ids=cores)
        ys = [res.results[c]["y_out"] for c in cores]
        last = (l == 3)
        maps = []
        for c in cores:
            b, half = c // 2, c % 2
            rows = slice(half * 2048, (half + 1) * 2048)
            yb = np.empty((2048, 4096), dtype=ys[0].dtype)
            for g in range(4):
                for hh in range(2):
                    yb[:, g * 1024 + hh * 512:g * 1024 + (hh + 1) * 512] = ys[2 * b + hh][rows, g * 512:(g + 1) * 512]
            m = {"x_in": xs[c], "y_in": yb, "gmix": col(I['mix_gain'][l]), "gffn": col(I['ffn_norm'][l]),
                 "w_out": I['w_out'][l], "w_g": I['w_ffn_gate'][l], "w_u": I['w_ffn_up'][l], "w_d": I['w_ffn_down'][l]}
            if last:
                m["gnext"] = col(I['final_norm'])
                m["gfin"] = np.ascontiguousarray(I['final_norm'].reshape(1, 4096).astype(np.float32))
            else:
                m["gnext"] = col(I['attn_norm'][l + 1])
            maps.append(m)
        res = run_bass_kernel_spmd(_prog('p3f' if last else 'p3'), maps, core_ids=cores)
        if last:
            out = np.concatenate([res.results[c]["out"] for c in cores], axis=0)
        else:
            xs = [res.results[c]["x_out"] for c in cores]
            hT = [res.results[c]["hT_out"] for c in cores]
    return out.reshape(4, 4096, 4096).astype(np.float32, copy=False)
```
